# Optimizing a Trainium2 kernel written in Bass

```python
import jax, jax.numpy as jnp
from jax import lax
import numpy as np

D_MODEL = 2048
BATCH = 2
SEQ = 16384
DEPTH = 2

D_MIX = D_MODEL
FOX_HEADS = 8
FOX_HEAD_DIM = 128
FOX_WIDTH = FOX_HEADS * FOX_HEAD_DIM
FOX_BLOCK = 128
RET_HEADS = 4
RET_HEAD_DIM = 128
RET_WIDTH = RET_HEADS * RET_HEAD_DIM
RET_CHUNK = 128
ROPE_BASE = 10000.0
RWKV_HEADS = 8
RWKV_HEAD_DIM = 64
RWKV_WIDTH = RWKV_HEADS * RWKV_HEAD_DIM
RWKV_DECAY_LORA = 96
RWKV_AAA_LORA = 96
RWKV_MV_LORA = 64
RWKV_GATE_LORA = 256
D_FF = 5632
NORM_EPS = 1e-6
RWKV_LNX_EPS = 64e-5

FOX_COLS = 4 * FOX_WIDTH + FOX_HEADS
RET_COLS = 4 * RET_WIDTH
RWKV_SHIFT_COLS = 3 * RWKV_WIDTH + RWKV_DECAY_LORA + RWKV_AAA_LORA + RWKV_GATE_LORA
N_IN_FIRST = FOX_COLS + RET_COLS + RWKV_SHIFT_COLS
N_IN_REST = N_IN_FIRST + RWKV_MV_LORA

kernel_name = 'hybrid_fox_retnet_rwkv7_macaron_sandwich'


def _split(z, sizes):
    idx = np.cumsum(sizes)[:-1].tolist()
    return jnp.split(z, idx, axis=-1)


def rms_norm(x, g, eps=NORM_EPS):
    xf = x.astype(jnp.float32)
    y = xf * lax.rsqrt(jnp.mean(xf * xf, axis=-1, keepdims=True) + eps)
    return (y * g.astype(jnp.float32)).astype(x.dtype)


def swiglu(u, w_gu, w_down):
    gate, up = jnp.split(u @ w_gu, 2, axis=-1)
    return (jax.nn.silu(gate) * up) @ w_down


def rotary(x, pos):
    half = x.shape[-1] // 2
    inv = ROPE_BASE ** (-jnp.arange(half, dtype=jnp.float32) / half)
    ang = pos[:, None] * inv[None, :]
    cos = jnp.cos(ang)[None, :, None, :]
    sin = jnp.sin(ang)[None, :, None, :]
    x1, x2 = x[..., :half], x[..., half:]
    return jnp.concatenate([x1 * cos - x2 * sin, x1 * sin + x2 * cos], axis=-1)


def forgetting_attention(p, qk_gain, f_bias):
    B, S, _ = p.shape
    f32 = jnp.float32
    q, k, v, og, f = _split(p, [FOX_WIDTH] * 4 + [FOX_HEADS])
    heads = lambda t: t.reshape(B, S, FOX_HEADS, FOX_HEAD_DIM).astype(f32).transpose(0, 2, 1, 3)
    q = rms_norm(heads(q), qk_gain[0]) * (FOX_HEAD_DIM ** -0.5)
    k = rms_norm(heads(k), qk_gain[1])
    v = heads(v)
    log_f = jax.nn.log_sigmoid(f.astype(f32) + f_bias.astype(f32))
    cum = jnp.cumsum(log_f, axis=1).transpose(0, 2, 1)
    nb = S // FOX_BLOCK
    q_blocks = q.reshape(B, FOX_HEADS, nb, FOX_BLOCK, FOX_HEAD_DIM).transpose(2, 0, 1, 3, 4)
    c_blocks = cum.reshape(B, FOX_HEADS, nb, FOX_BLOCK).transpose(2, 0, 1, 3)
    key_pos = jnp.arange(S)

    def attend(args):
        qb, cb, start = args
        logits = jnp.einsum('bhqd,bhkd->bhqk', qb, k) + cb[..., None] - cum[:, :, None, :]
        q_pos = start + jnp.arange(FOX_BLOCK)
        logits = jnp.where(key_pos[None, :] <= q_pos[:, None], logits, -jnp.inf)
        probs = jax.nn.softmax(logits, axis=-1)
        return jnp.einsum('bhqk,bhkd->bhqd', probs, v)

    o = lax.map(attend, (q_blocks, c_blocks, jnp.arange(nb) * FOX_BLOCK))
    o = o.transpose(1, 0, 3, 2, 4).reshape(B, S, FOX_WIDTH)
    return (o * jax.nn.sigmoid(og.astype(f32))).astype(p.dtype)


def retention(p):
    B, S, _ = p.shape
    f32 = jnp.float32
    H, Dh, C = RET_HEADS, RET_HEAD_DIM, RET_CHUNK
    q, k, v, g = _split(p, [RET_WIDTH] * 4)
    heads = lambda t: t.reshape(B, S, H, Dh).astype(f32)
    pos = jnp.arange(S, dtype=f32)
    q = rotary(heads(q), pos)
    k = rotary(heads(k), pos) * (Dh ** -0.5)
    v = heads(v)
    lg = jnp.log(1.0 - 2.0 ** (-5.0 - jnp.arange(H, dtype=f32)))
    n = S // C
    qc = q.reshape(B, n, C, H, Dh)
    kc = k.reshape(B, n, C, H, Dh)
    vc = v.reshape(B, n, C, H, Dh)
    idx = jnp.arange(C, dtype=f32)
    diff = idx[:, None] - idx[None, :]
    decay_in = jnp.where(diff >= 0, jnp.exp(jnp.maximum(diff, 0.0)[None] * lg[:, None, None]), 0.0)
    scores = jnp.einsum('bnihd,bnjhd->bnhij', qc, kc) * decay_in
    inner = jnp.einsum('bnhij,bnjhe->bnihe', scores, vc)
    q_decay = jnp.exp((idx[:, None] + 1.0) * lg[None, :])
    k_decay = jnp.exp((C - 1.0 - idx)[:, None] * lg[None, :])
    chunk_decay = jnp.exp(C * lg)
    chunk_kv = jnp.einsum('bnjhd,bnjhe->nbhde', kc * k_decay[:, :, None], vc)

    def step(state, kv):
        return state * chunk_decay[None, :, None, None] + kv, state

    _, prev = lax.scan(step, jnp.zeros((B, H, Dh, Dh), f32), chunk_kv)
    cross = jnp.einsum('bnihd,nbhde->bnihe', qc * q_decay[:, :, None], prev)
    o = (inner + cross).reshape(B, S, H, Dh)
    o = o * lax.rsqrt(jnp.mean(o * o, axis=-1, keepdims=True) + NORM_EPS)
    o = o.reshape(B, S, RET_WIDTH)
    return (jax.nn.silu(g.astype(f32)) * o).astype(p.dtype)


def token_shift(z, mu):
    z_prev = jnp.pad(z, ((0, 0), (1, 0), (0, 0)))[:, :-1]
    return z + (z_prev - z) * mu


def rwkv7_time_mix(p, mu, vec, w2, a2, g2, r_k, v_res):
    B, S, _ = p.shape
    f32 = jnp.float32
    H, N, W = RWKV_HEADS, RWKV_HEAD_DIM, RWKV_WIDTH
    z = token_shift(p.astype(f32), mu.astype(f32))
    r, k, v, wl, al, gl = _split(z, [W, W, W, RWKV_DECAY_LORA, RWKV_AAA_LORA, RWKV_GATE_LORA])
    vec = vec.astype(f32)
    w0, a0, k_k, k_a, lnx_w, lnx_b = vec[0], vec[1], vec[2], vec[3], vec[4], vec[5]
    log_decay = -jnp.exp(-jax.nn.softplus(-(w0 + jnp.tanh(wl) @ w2)) - 0.5)
    a = jax.nn.sigmoid(a0 + al @ a2)
    g = jax.nn.sigmoid(gl) @ g2
    v_layer = v
    if v_res is not None:
        p_vres, v0, v2, v_first = v_res
        v = v + (v_first - v) * jax.nn.sigmoid(v0 + p_vres.astype(f32) @ v2)
    heads = lambda t: t.reshape(B, S, H, N)
    kk = heads(k * k_k)
    kk = kk / jnp.maximum(jnp.sqrt(jnp.sum(kk * kk, axis=-1, keepdims=True)), 1e-12)
    k = k * (1.0 + (a - 1.0) * k_a)
    rh, kh, vh, ah = heads(r), heads(k), heads(v), heads(a)
    decay = jnp.exp(heads(log_decay))
    tm = lambda t: jnp.moveaxis(t, 1, 0)

    def step(state, inp):
        r_t, w_t, k_t, v_t, a_t, b_t = inp
        sa = jnp.einsum('bhvk,bhk->bhv', state, a_t)
        state = state * w_t[:, :, None, :] + sa[..., None] * b_t[:, :, None, :] + v_t[..., None] * k_t[:, :, None, :]
        return state, jnp.einsum('bhvk,bhk->bhv', state, r_t)

    _, y = lax.scan(step, jnp.zeros((B, H, N, N), f32),
                    (tm(rh), tm(decay), tm(kh), tm(vh), tm(-kk), tm(kk * ah)))
    y = jnp.moveaxis(y, 0, 1)
    mean = jnp.mean(y, axis=-1, keepdims=True)
    var = jnp.mean(jnp.square(y - mean), axis=-1, keepdims=True)
    y = ((y - mean) * lax.rsqrt(var + RWKV_LNX_EPS)).reshape(B, S, W) * lnx_w + lnx_b
    bonus = jnp.sum(rh * kh * r_k.astype(f32), axis=-1, keepdims=True) * vh
    y = (y + bonus.reshape(B, S, W)) * g
    return y.astype(p.dtype), v_layer


def setup_inputs(seed: int = 0) -> dict:
    key = jax.random.key(seed)
    ks = jax.random.split(key, 20)
    f32 = jnp.float32
    nrm = jax.random.normal
    W = RWKV_WIDTH
    x = nrm(ks[0], (BATCH, SEQ, D_MODEL), f32)
    norm_gains = 1.0 + 0.05 * nrm(ks[1], (DEPTH, 6, D_MODEL), f32)
    ffn_w_gu = nrm(ks[2], (DEPTH, 2, D_MODEL, 2 * D_FF), f32) * D_MODEL ** -0.5
    ffn_w_down = nrm(ks[3], (DEPTH, 2, D_FF, D_MODEL), f32) * D_FF ** -0.5
    w_in_first = nrm(ks[4], (D_MODEL, N_IN_FIRST), f32) * D_MODEL ** -0.5
    w_in_rest = nrm(ks[5], (DEPTH - 1, D_MODEL, N_IN_REST), f32) * D_MODEL ** -0.5
    w_out = nrm(ks[6], (DEPTH, D_MIX, D_MODEL), f32) * D_MIX ** -0.5
    fox_qk_gain = 1.0 + 0.05 * nrm(ks[7], (DEPTH, 2, FOX_HEAD_DIM), f32)
    fox_f_bias = 2.0 + 0.5 * nrm(ks[8], (DEPTH, FOX_HEADS), f32)
    rwkv_mu = jax.random.uniform(ks[9], (DEPTH, RWKV_SHIFT_COLS), f32)
    vec_off = jnp.array([0.0, 0.0, 0.85, 1.0, 1.0, 0.0], f32)
    vec_scale = jnp.array([0.5, 0.1, 0.05, 0.05, 0.05, 0.01], f32)
    rwkv_vec = vec_off[None, :, None] + vec_scale[None, :, None] * nrm(ks[10], (DEPTH, 6, W), f32)
    rwkv_w2 = 0.1 * nrm(ks[11], (DEPTH, RWKV_DECAY_LORA, W), f32) * RWKV_DECAY_LORA ** -0.5
    rwkv_a2 = 0.1 * nrm(ks[12], (DEPTH, RWKV_AAA_LORA, W), f32) * RWKV_AAA_LORA ** -0.5
    rwkv_g2 = nrm(ks[13], (DEPTH, RWKV_GATE_LORA, W), f32) * RWKV_GATE_LORA ** -0.5
    rwkv_r_k = 0.1 * nrm(ks[14], (DEPTH, RWKV_HEADS, RWKV_HEAD_DIM), f32)
    rwkv_v0 = 1.0 + 0.1 * nrm(ks[15], (DEPTH - 1, W), f32)
    rwkv_v2 = 0.1 * nrm(ks[16], (DEPTH - 1, RWKV_MV_LORA, W), f32) * RWKV_MV_LORA ** -0.5
    return {'x': x, 'norm_gains': norm_gains, 'ffn_w_gu': ffn_w_gu, 'ffn_w_down': ffn_w_down,
            'w_in_first': w_in_first, 'w_in_rest': w_in_rest, 'w_out': w_out,
            'fox_qk_gain': fox_qk_gain, 'fox_f_bias': fox_f_bias, 'rwkv_mu': rwkv_mu,
            'rwkv_vec': rwkv_vec, 'rwkv_w2': rwkv_w2, 'rwkv_a2': rwkv_a2, 'rwkv_g2': rwkv_g2,
            'rwkv_r_k': rwkv_r_k, 'rwkv_v0': rwkv_v0, 'rwkv_v2': rwkv_v2}


def reference(x, norm_gains, ffn_w_gu, ffn_w_down, w_in_first, w_in_rest, w_out,
              fox_qk_gain, fox_f_bias, rwkv_mu, rwkv_vec, rwkv_w2, rwkv_a2, rwkv_g2,
              rwkv_r_k, rwkv_v0, rwkv_v2):
    h = x
    v_first = None
    for l in range(DEPTH):
        g = norm_gains[l]
        h = h + 0.5 * rms_norm(swiglu(rms_norm(h, g[0]), ffn_w_gu[l, 0], ffn_w_down[l, 0]), g[1])
        u = rms_norm(h, g[2])
        if l == 0:
            p_fox, p_ret, p_rwkv = _split(u @ w_in_first, [FOX_COLS, RET_COLS, RWKV_SHIFT_COLS])
            v_res = None
        else:
            p_fox, p_ret, p_rwkv, p_vres = _split(u @ w_in_rest[l - 1],
                                                  [FOX_COLS, RET_COLS, RWKV_SHIFT_COLS, RWKV_MV_LORA])
            v_res = (p_vres, rwkv_v0[l - 1], rwkv_v2[l - 1], v_first)
        y_fox = forgetting_attention(p_fox, fox_qk_gain[l], fox_f_bias[l])
        y_ret = retention(p_ret)
        y_rwkv, v_layer = rwkv7_time_mix(p_rwkv, rwkv_mu[l], rwkv_vec[l], rwkv_w2[l], rwkv_a2[l],
                                         rwkv_g2[l], rwkv_r_k[l], v_res)
        if l == 0:
            v_first = v_layer
        y = jnp.concatenate([y_fox, y_ret, y_rwkv], axis=-1) @ w_out[l]
        h = h + rms_norm(y, g[3])
        h = h + 0.5 * rms_norm(swiglu(rms_norm(h, g[4]), ffn_w_gu[l, 1], ffn_w_down[l, 1]), g[5])
    return h
```

```python
from contextlib import ExitStack
import numpy as np
import concourse.bass as bass
import concourse.mybir as mybir
from concourse.bass_utils import run_bass_kernel_spmd

F32 = mybir.dt.float32
BF16 = mybir.dt.bfloat16
AF = mybir.ActivationFunctionType
ALU = mybir.AluOpType

DMIX = 2048
KM = 16
T = 512
EPS = 1e-6
FOXW = 1024
RET0 = 4104
RW0 = 6152
N_IN0 = 8136
N_IN1 = 8200

def _fm_chunks(layer):
    ch = []
    for i in range(8):
        ch.append(("fq%d" % i, i * 128, 128))
    for i in range(8):
        ch.append(("fk%d" % i, 1024 + i * 128, 128))
    for i in range(8):
        ch.append(("fg%d" % i, 3072 + i * 128, 128))
    ch.append(("ff", 4096, 8))
    for i in range(4):
        ch.append(("rq%d" % i, RET0 + i * 128, 128))
    for i in range(4):
        ch.append(("rk%d" % i, RET0 + 512 + i * 128, 128))
    for i in range(4):
        ch.append(("rg%d" % i, RET0 + 1536 + i * 128, 128))
    for i in range(4):
        ch.append(("wr%d" % i, RW0 + i * 128, 128))
    for i in range(4):
        ch.append(("wk%d" % i, RW0 + 512 + i * 128, 128))
    for i in range(4):
        ch.append(("wv%d" % i, RW0 + 1024 + i * 128, 128))
    ch.append(("wwl", RW0 + 1536, 96))
    ch.append(("wal", RW0 + 1632, 96))
    ch.append(("wgl0", RW0 + 1728, 128))
    ch.append(("wgl1", RW0 + 1856, 128))
    if layer > 0:
        ch.append(("wvr", N_IN0, 64))
    return ch

NCH = 54
TM_TILES = [2048, 2560, RET0 + 1024]


class ChunkedRows:
    def __init__(self, aps):
        self.aps = aps

    def __getitem__(self, key):
        rows, cols = key
        c, r0 = rows.start // 128, rows.start % 128
        return self.aps[c][r0:r0 + (rows.stop - rows.start), cols]


class Sched:
    def __init__(self, nc, es):
        self.nc = nc
        self.eng = {"pe": nc.tensor, "act": nc.scalar, "dve": nc.vector, "pool": nc.gpsimd, "sp": nc.sync}
        self.sem = {}
        self.cnt = {}
        self.seen = {e: {} for e in self.eng}
        self.buf = {}
        for e in ("pe", "act", "dve", "pool"):
            self.sem[e] = es.enter_context(nc.semaphore("sem_" + e))
            self.cnt[e] = 0
        self.lanes = {"sp": [], "pool": [], "act": []}
        self.lane_rr = {"sp": 0, "pool": 0, "act": 0}
        for q, n in (("sp", 6), ("pool", 4), ("act", 2)):
            for i in range(n):
                name = "dq_%s%d" % (q, i)
                self.sem[name] = es.enter_context(nc.semaphore(name))
                self.cnt[name] = 0
                self.lanes[q].append(name)

    def _wait(self, e, src, val):
        if val <= 0:
            return
        if self.seen[e].get(src, 0) >= val:
            return
        self.eng[e].wait_ge(self.sem[src], val)
        self.seen[e][src] = val

    def _deps(self, e, reads, writes):
        deps = {}
        for k in reads:
            b = self.buf.get(k)
            if b is not None and b[0] is not None:
                s, v = b[0]
                deps[s] = max(deps.get(s, 0), v)
            if b is not None and isinstance(k, str) and k[:2] == "ps" and k[2:].isdigit():
                for s, v in b[1].items():
                    if s != e:
                        deps[s] = max(deps.get(s, 0), v)
        for k in writes:
            b = self.buf.get(k)
            if b is not None:
                if b[0] is not None:
                    s, v = b[0]
                    deps[s] = max(deps.get(s, 0), v)
                for s, v in b[1].items():
                    deps[s] = max(deps.get(s, 0), v)
        for s, v in deps.items():
            if s == "pe" and e == "pe":
                continue
            self._wait(e, s, v)

    def _mark(self, src, val, reads, writes):
        for k in reads:
            b = self.buf.setdefault(k, [None, {}])
            b[1][src] = val
        for k in writes:
            self.buf[k] = [(src, val), {}]

    def op(self, e, emit, reads=(), writes=()):
        self._deps(e, reads, writes)
        ins = emit()
        ins.then_inc(self.sem[e], 1)
        self.cnt[e] += 1
        self._mark(e, self.cnt[e], reads, writes)
        return ins

    def dma(self, q, out, in_, reads=(), writes=()):
        lanes = self.lanes[q]
        lane = lanes[self.lane_rr[q] % len(lanes)]
        self.lane_rr[q] += 1
        self._wait(q, lane, self.cnt[lane])
        self._deps(q, reads, writes)
        ins = self.eng[q].dma_start(out=out, in_=in_)
        ins.then_inc(self.sem[lane], 16)
        self.cnt[lane] += 16
        self._mark(lane, self.cnt[lane], reads, writes)
        return ins

    def drain(self, e):
        for s, v in self.cnt.items():
            self._wait(e, s, v)


class Builder:
    def __init__(self, S, nq, n_layers=2, debug=False, stop_after=None, D=2048, DFF=5632, mixers=("fox", "ret", "rwkv")):
        self.mixers = mixers
        self.S = S
        self.D, self.DFF, self.KC, self.JC = D, DFF, D // 128, DFF // 128
        self.nq = nq
        self.L = n_layers
        self.NT = S // T
        self.debug = debug
        self.stop_after = stop_after
        self.nc = bass.Bass("TRN2", target_bir_lowering=False)
        self.es = ExitStack()
        self.rr = 0

    def dram_in(self, name, shape):
        return self.nc.dram_tensor(name, list(shape), F32, kind="ExternalInput").ap()

    def dram_tmp(self, name, shape, dt, out=False):
        kind = "ExternalOutput" if (out and self.debug) else "Internal"
        return self.nc.dram_tensor(name, list(shape), dt, kind=kind).ap()

    def sb(self, st, name, shape, dt):
        self.uid = getattr(self, "uid", 0) + 1
        return st.enter_context(self.nc.sbuf_tensor("%s_u%d" % (name, self.uid), list(shape), dt))

    def alt(self):
        self.rr += 1
        return "dve" if self.rr % 2 else "act"

    def copy(self, e, out, in_, reads, writes):
        nc = self.nc
        if e == "act":
            return self.sc.op("act", lambda: nc.scalar.copy(out=out, in_=in_), reads, writes)
        if e == "pool":
            return self.sc.op("pool", lambda: nc.gpsimd.tensor_copy(out=out, in_=in_), reads, writes)
        return self.sc.op("dve", lambda: nc.vector.tensor_copy(out=out, in_=in_), reads, writes)

    def build(self):
        D, KC, DFF, JC = self.D, self.KC, self.DFF, self.JC
        nc, es, S = self.nc, self.es, self.S
        L = self.L
        I = {}
        I["x"] = self.dram_in("x", [S, D])
        I["norm_gains"] = self.dram_in("norm_gains", [2, 6, D])
        I["ffn_w_gu"] = self.dram_in("ffn_w_gu", [2, 2, D, 2 * DFF])
        I["ffn_w_down"] = self.dram_in("ffn_w_down", [2, 2, DFF, D])
        I["w_in_first"] = self.dram_in("w_in_first", [D, N_IN0])
        I["w_in_rest"] = self.dram_in("w_in_rest", [1, D, N_IN1])
        I["w_out"] = self.dram_in("w_out", [2, DMIX, D])
        I["fox_qk_gain"] = self.dram_in("fox_qk_gain", [2, 2, 128])
        I["fox_f_bias"] = self.dram_in("fox_f_bias", [2, 8])
        I["rwkv_mu"] = self.dram_in("rwkv_mu", [2, 1984])
        I["rwkv_vec"] = self.dram_in("rwkv_vec", [2, 6, 512])
        I["rwkv_w2"] = self.dram_in("rwkv_w2", [2, 96, 512])
        I["rwkv_a2"] = self.dram_in("rwkv_a2", [2, 96, 512])
        I["rwkv_g2"] = self.dram_in("rwkv_g2", [2, 256, 512])
        I["rwkv_r_k"] = self.dram_in("rwkv_r_k", [2, 8, 64])
        I["rwkv_v0"] = self.dram_in("rwkv_v0", [1, 512])
        I["rwkv_v2"] = self.dram_in("rwkv_v2", [1, 64, 512])
        I["cst"] = self.dram_in("cst", [128, CST_W])
        I["cmix"] = self.dram_in("cmix", [128, CM_W])
        I["rot"] = self.dram_in("rot", [2, 128, S])
        self.I = I
        rows_out = S // self.nq
        self.out = nc.dram_tensor("out", [rows_out, D], F32, kind="ExternalOutput").ap()
        self.wgu = [[self.dram_tmp("wgu_%d_%d" % (l, f), [JC, 128, KC, 256], BF16) for f in range(2)] for l in range(L)]
        self.wdn = [[self.dram_tmp("wdn_%d_%d" % (l, f), [KC, 128, JC, 128], BF16) for f in range(2)] for l in range(L)]
        self.win = [self.dram_tmp("win_%d" % l, [NCH, 128, KC, 128], BF16) for l in range(L)]
        self.wvm = [self.dram_tmp("wvm_%d" % l, [3, 128, KC, 512], BF16) for l in range(L)]
        self.wo = [self.dram_tmp("wo_%d" % l, [KC, 128, KM, 128], BF16) for l in range(L)]
        self.h1T = self.dram_tmp("h1T", [D, S], F32, out=True)
        self.pT = ChunkedRows([self.dram_tmp("pT%d" % i, [128, S], F32, out=True) for i in range(NCH)])
        self.vtok = self.dram_tmp("vtok", [S, 1536], BF16, out=True)
        self.ycatT = self.dram_tmp("ycatT", [DMIX, S], BF16, out=True)
        self.vfirstT = self.dram_tmp("vfirstT", [512, S], F32)
        self.cumT = self.dram_tmp("cumT", [8, S], F32)

        self.sc = Sched(nc, es)
        sc = self.sc
        self.ps = [es.enter_context(nc.psum_tensor("ps%d" % i, [128, 512], F32)) for i in range(8)]
        self.cst = self.sb(es, "cst_sb", [128, CST_W], F32)
        sc.dma("sp", self.cst[:], I["cst"][:, :], writes=["cst"])
        self.ident = self.cst[:, C_ID:C_ID + 128]
        self.one_col = self.cst[:, C_ONE:C_ONE + 1]
        self.ones_bf = self.sb(es, "ones_bf", [128, 128], BF16)
        sc.op("pool", lambda: nc.gpsimd.memset(self.ones_bf[:], 1.0), writes=["ones_bf"])
        self.gcol = self.sb(es, "gcol", [128, 12, KC], F32)
        self.ghalf = self.sb(es, "ghalf", [128, 12, KC], F32)
        with nc.allow_non_contiguous_dma(reason="tiny gain vectors, column layout"):
            for gl in range(2):
                for gi in range(6):
                    sc.dma("pool", self.gcol[:, gl * 6 + gi, :], I["norm_gains"][gl, gi].rearrange("(c p) -> p c", p=128))
            for lane in sc.lanes["pool"]:
                sc._wait("dve", lane, sc.cnt[lane])
        sc.op("dve", lambda: nc.vector.tensor_scalar(out=self.ghalf[:], in0=self.gcol[:], scalar1=0.5, scalar2=None,
                                                      op0=ALU.mult), reads=["gcol"], writes=["ghalf"])
        self.prep_weights()
        if self.stop_after == "prep":
            return self.finish()
        for l in range(L):
            if l == 0:
                self.token_phase(first=True, layer=0)
            if self.stop_after == "A%d" % l:
                return self.finish()
            self.mixer_phase(l)
            if self.stop_after == "B%d" % l:
                return self.finish()
            self.token_phase(first=False, layer=l)
        return self.finish()

    def finish(self):
        for e in ("sp", "pool", "act", "dve", "pe"):
            self.sc.drain(e)
        self.es.close()
        return self.nc

    def prep_one(self, st, W, Kdim, col0, ncols, dst, tw, dcol0=0):
        nc, sc = self.nc, self.sc
        kcs = Kdim // 128
        CB = 2048
        for kc in range(kcs):
            c = 0
            while c < ncols:
                cb = min(CB, ncols - c)
                i = self.pp % 2
                self.pp += 1
                sf, sbf = self.pstg[i], self.pstb[i]
                sc.dma("sp", sf[:, 0:cb], W[kc * 128:(kc + 1) * 128, col0 + c:col0 + c + cb], writes=["pstg%d" % i])
                eng = ("dve", "act", "pool")[self.pp % 3]
                self.copy(eng, sbf[:, 0:cb], sf[:, 0:cb], ["pstg%d" % i], ["pstb%d" % i])
                t0 = c // tw
                nfull = cb // tw
                if nfull > 0:
                    sc.dma("pool", dst[t0:t0 + nfull, :, kc, dcol0:dcol0 + tw].rearrange("t p c -> p t c"),
                           sbf[:, 0:nfull * tw].rearrange("p (t c) -> p t c", c=tw), reads=["pstb%d" % i])
                rem = cb - nfull * tw
                if rem > 0:
                    sc.dma("pool", dst[t0 + nfull, :, kc, dcol0:dcol0 + rem], sbf[:, nfull * tw:cb],
                           reads=["pstb%d" % i])
                c += cb

    def prep_weights(self):
        D, KC, DFF, JC = self.D, self.KC, self.DFF, self.JC
        nc, sc, I = self.nc, self.sc, self.I
        self.pp = 0
        with ExitStack() as st:
            self.pstg = [self.sb(st, "pstg%d" % i, [128, 2048], F32) for i in range(2)]
            self.pstb = [self.sb(st, "pstb%d" % i, [128, 2048], BF16) for i in range(2)]
            with nc.allow_non_contiguous_dma(reason="weight re-tiling, 256B+ runs"):
                for l in range(self.L):
                    for f in range(2):
                        Wgu = I["ffn_w_gu"][l, f]
                        self.prep_one(st, Wgu, D, 0, DFF, self.wgu[l][f], 128, 0)
                        self.prep_one(st, Wgu, D, DFF, DFF, self.wgu[l][f], 128, 128)
                        self.prep_one(st, I["ffn_w_down"][l, f], DFF, 0, D, self.wdn[l][f], 128, 0)
                    Win = I["w_in_first"] if l == 0 else I["w_in_rest"][l - 1]
                    for ci, (nm, c0, w) in enumerate(_fm_chunks(l)):
                        self.prep_one(st, Win, D, c0, w, self.win[l][ci:ci + 1], 128, 0)
                    for ti, c0 in enumerate(TM_TILES):
                        self.prep_one(st, Win, D, c0, 512, self.wvm[l][ti:ti + 1], 512, 0)
                    self.prep_one(st, I["w_out"][l], DMIX, 0, D, self.wo[l], 128, 0)
            self.barrier()

    def barrier(self):
        for e in ("sp", "pool", "act", "dve", "pe"):
            self.sc.drain(e)

    def load_w(self, src, nelem, shape3):
        i = self.wrr % len(self.wsl)
        self.wrr += 1
        key = "wsl%d" % i
        a, b = shape3
        ap = self.wsl[i][:, 0:a * b].rearrange("p (a b) -> p a b", b=b)
        self.sc.dma("sp", ap, src, writes=[key])
        return ap, key

    def stats_finish(self, ps_key, ps_ap, dim):
        nc, sc = self.nc, self.sc
        sc.op("dve", lambda: nc.vector.tensor_scalar(out=self.ms[:], in0=ps_ap, scalar1=1.0 / dim, scalar2=EPS,
                                                      op0=ALU.mult, op1=ALU.add), reads=[ps_key], writes=["ms"])
        sc.op("act", lambda: nc.scalar.activation(out=self.ms[:], in_=self.ms[:], func=AF.Sqrt), reads=["ms"], writes=["ms"])
        sc.op("dve", lambda: nc.vector.reciprocal(out=self.rstd[:], in_=self.ms[:]), reads=["ms"], writes=["rstd"])

    def norm_to_uT(self, gi):
        D, KC, DFF, JC = self.D, self.KC, self.DFF, self.JC
        nc, sc = self.nc, self.sc
        for kc in range(KC):
            i = kc % 2
            sc.op("act", lambda: nc.scalar.activation(out=self.sq[i][:], in_=self.hT[:, kc, :], func=AF.Square),
                  reads=["hT%d" % kc], writes=["sq%d" % i])
            sc.op("pe", lambda: nc.tensor.matmul(self.ps[6][:], lhsT=self.ones_bf[:], rhs=self.sq[i][:],
                                                  start=(kc == 0), stop=(kc == KC - 1)),
                  reads=["sq%d" % i, "ones_bf"], writes=["ps6"])
        self.stats_finish("ps6", self.ps[6][:], D)
        for kc in range(KC):
            sc.op("dve", lambda: nc.vector.scalar_tensor_tensor(out=self.uT[:, kc, :], in0=self.hT[:, kc, :],
                                                                 scalar=self.gcol[:, gi, kc:kc + 1], in1=self.rstd[:],
                                                                 op0=ALU.mult, op1=ALU.mult),
                  reads=["hT%d" % kc, "rstd", "gcol"], writes=["uT"])

    def resid_update(self, gi, half):
        D, KC, DFF, JC = self.D, self.KC, self.DFF, self.JC
        nc, sc = self.nc, self.sc
        self.stats_finish("ps6", self.ps[6][:], D)
        g = self.ghalf if half else self.gcol
        for m in range(KC):
            sc.op("dve", lambda: nc.vector.scalar_tensor_tensor(out=self.fT[:, m, :], in0=self.fT[:, m, :],
                                                                 scalar=g[:, gi, m:m + 1], in1=self.rstd[:],
                                                                 op0=ALU.mult, op1=ALU.mult),
                  reads=["fT%d" % m, "rstd", "gcol", "ghalf"], writes=["fT%d" % m])
            sc.op("pool", lambda: nc.gpsimd.tensor_tensor(out=self.hT[:, m, :], in0=self.hT[:, m, :], in1=self.fT[:, m, :],
                                                          op=ALU.add),
                  reads=["fT%d" % m, "hT%d" % m], writes=["hT%d" % m])

    def proj_to_fT(self, wt, kcs, inT, in_key):
        D, KC, DFF, JC = self.D, self.KC, self.DFF, self.JC
        nc, sc = self.nc, self.sc
        for m in range(KC):
            w, wk = self.load_w(wt[m], kcs * 128, (kcs, 128))
            pb = 4 + (m % 2)
            for k in range(kcs):
                sc.op("pe", lambda: nc.tensor.matmul(self.ps[pb][:], lhsT=w[:, k, :], rhs=inT[:, k, :],
                                                      start=(k == 0), stop=(k == kcs - 1)),
                      reads=[wk, in_key], writes=["ps%d" % pb])
            sc.op("dve", lambda: nc.vector.tensor_copy(out=self.fT[:, m, :], in_=self.ps[pb][:]),
                  reads=["ps%d" % pb], writes=["fT%d" % m])
            i = m % 2
            sc.op("act", lambda: nc.scalar.activation(out=self.sq[i][:], in_=self.fT[:, m, :], func=AF.Square),
                  reads=["fT%d" % m], writes=["sq%d" % i])
            sc.op("pe", lambda: nc.tensor.matmul(self.ps[6][:], lhsT=self.ones_bf[:], rhs=self.sq[i][:],
                                                  start=(m == 0), stop=(m == KC - 1)),
                  reads=["sq%d" % i, "ones_bf"], writes=["ps6"])

    def ffn(self, l, f, g_in, g_out):
        D, KC, DFF, JC = self.D, self.KC, self.DFF, self.JC
        nc, sc = self.nc, self.sc
        self.norm_to_uT(g_in)
        wgu = self.wgu[l][f]
        for j in range(JC):
            w, wk = self.load_w(wgu[j], KC * 256, (KC, 256))
            pg, pu = (j % 2), 2 + (j % 2)
            for k in range(KC):
                sc.op("pe", lambda: nc.tensor.matmul(self.ps[pg][:], lhsT=w[:, k, 0:128], rhs=self.uT[:, k, :],
                                                      start=(k == 0), stop=(k == KC - 1)),
                      reads=[wk, "uT"], writes=["ps%d" % pg])
            for k in range(KC):
                sc.op("pe", lambda: nc.tensor.matmul(self.ps[pu][:], lhsT=w[:, k, 128:256], rhs=self.uT[:, k, :],
                                                      start=(k == 0), stop=(k == KC - 1)),
                      reads=[wk, "uT"], writes=["ps%d" % pu])
            i = j % 2
            sc.op("act", lambda: nc.scalar.activation(out=self.sg[i][:], in_=self.ps[pg][:], func=AF.Silu),
                  reads=["ps%d" % pg], writes=["sg%d" % i])
            sc.op("dve", lambda: nc.vector.tensor_tensor(out=self.actT[:, j, :], in0=self.sg[i][:], in1=self.ps[pu][:],
                                                          op=ALU.mult),
                  reads=["sg%d" % i, "ps%d" % pu], writes=["actT"])
        import os
        bis = int(os.environ.get("BISECT", "99"))
        if bis == 4:
            return
        self.proj_to_fT(self.wdn[l][f], JC, self.actT, "actT")
        if bis == 5:
            return
        self.resid_update(g_out, True)

    def in_proj(self, l, t0):
        D, KC, DFF, JC = self.D, self.KC, self.DFF, self.JC
        nc, sc = self.nc, self.sc
        chunks = _fm_chunks(l)
        for ci, (nm, c0, wd) in enumerate(chunks):
            w, wk = self.load_w(self.win[l][ci][:, :, 0:wd], KC * wd, (KC, wd))
            pb = ci % 4
            for k in range(KC):
                sc.op("pe", lambda: nc.tensor.matmul(self.ps[pb][0:wd, :], lhsT=w[:, k, 0:wd], rhs=self.uT[:, k, :],
                                                      start=(k == 0), stop=(k == KC - 1)),
                      reads=[wk, "uT"], writes=["ps%d" % pb])
            i = ci % 2
            self.copy(self.alt(), self.stg[i][0:wd, :], self.ps[pb][0:wd, :], ["ps%d" % pb], ["stg%d" % i])
            sc.dma("pool", self.pT[ci * 128:ci * 128 + wd, t0:t0 + T], self.stg[i][0:wd, :], reads=["stg%d" % i],
                   writes=[("pT", ci, t0 // T)])
        for ti in range(3):
            w, wk = self.load_w(self.wvm[l][ti], KC * 512, (KC, 512))
            for tb in range(T // 128):
                pb = (ti * 4 + tb) % 4
                for k in range(KC):
                    sc.op("pe", lambda: nc.tensor.matmul(self.ps[pb][:], lhsT=self.uT[:, k, tb * 128:(tb + 1) * 128],
                                                          rhs=w[:, k, :], start=(k == 0), stop=(k == KC - 1)),
                          reads=[wk, "uT"], writes=["ps%d" % pb])
                i = (ti * 4 + tb) % 2
                self.copy(self.alt(), self.stgb[i][:], self.ps[pb][:], ["ps%d" % pb], ["stgb%d" % i])
                sc.dma("pool", self.vtok[t0 + tb * 128:t0 + (tb + 1) * 128, ti * 512:(ti + 1) * 512], self.stgb[i][:],
                       reads=["stgb%d" % i], writes=[("vtok", t0 // T)])

    def load_x_tile(self, t0):
        D, KC, DFF, JC = self.D, self.KC, self.DFF, self.JC
        nc, sc = self.nc, self.sc
        for tb in range(T // 128):
            i = tb % 2
            sc.dma("pool", self.xin[i][:], self.I["x"][t0 + tb * 128:t0 + (tb + 1) * 128, :], writes=["xin%d" % i])
            for fg in range(KC // 4):
                pb = fg % 4
                for q in range(4):
                    fc = fg * 4 + q
                    sc.op("pe", lambda: nc.tensor.transpose(out=self.ps[pb][:, q * 128:(q + 1) * 128],
                                                             in_=self.xin[i][:, fc * 128:(fc + 1) * 128], identity=self.ident),
                          reads=["xin%d" % i, "cst"], writes=["ps%d" % pb])
                self.copy(self.alt(), self.hT[:, fg * 4:fg * 4 + 4, tb * 128:(tb + 1) * 128],
                          self.ps[pb][:].rearrange("p (q t) -> p q t", q=4), ["ps%d" % pb],
                          ["hT%d" % (fg * 4 + q) for q in range(4)])

    def store_out_tile(self, r0):
        D, KC, DFF, JC = self.D, self.KC, self.DFF, self.JC
        nc, sc = self.nc, self.sc
        for tb in range(T // 128):
            i = tb % 2
            for fg in range(KC // 4):
                pb = fg % 4
                for q in range(4):
                    fc = fg * 4 + q
                    sc.op("pe", lambda: nc.tensor.transpose(out=self.ps[pb][:, q * 128:(q + 1) * 128],
                                                             in_=self.hT[:, fc, tb * 128:(tb + 1) * 128], identity=self.ident),
                          reads=["hT%d" % fc, "cst"], writes=["ps%d" % pb])
                self.copy(self.alt(), self.xin[i][:, fg * 512:(fg + 1) * 512], self.ps[pb][:], ["ps%d" % pb], ["xin%d" % i])
            sc.dma("pool", self.out[r0 + tb * 128:r0 + (tb + 1) * 128, :], self.xin[i][:], reads=["xin%d" % i], writes=["out"])

    def token_phase(self, first, layer):
        D, KC, DFF, JC = self.D, self.KC, self.DFF, self.JC
        nc, sc, S = self.nc, self.sc, self.S
        last = (not first) and (layer == self.L - 1)
        with ExitStack() as st:
            self.hT = self.sb(st, "hT", [128, KC, T], F32)
            self.uT = self.sb(st, "uT", [128, max(KC, KM), T], BF16)
            self.actT = self.sb(st, "actT", [128, JC, T], BF16)
            self.fT = self.sb(st, "fT", [128, KC, T], F32)
            self.wsl = [self.sb(st, "wsl%d" % i, [128, 8192], BF16) for i in range(3)]
            self.wrr = 0
            self.stg = [self.sb(st, "stg%d" % i, [128, T], F32) for i in range(2)]
            self.stgb = [self.sb(st, "stgb%d" % i, [128, T], BF16) for i in range(2)]
            self.sq = [self.sb(st, "sq%d" % i, [128, T], BF16) for i in range(2)]
            self.sg = [self.sb(st, "sg%d" % i, [128, T], F32) for i in range(2)]
            self.rstd = self.sb(st, "rstd", [128, T], F32)
            self.ms = self.sb(st, "ms", [128, T], F32)
            self.xin = [self.sb(st, "xin%d" % i, [128, D], F32) for i in range(2)]
            hkeys = ["hT%d" % k for k in range(KC)]
            if last:
                ntiles = (S // self.nq) // T
                if self.nq > 1:
                    pid = nc.gpsimd.partition_id()
                    base = (pid % self.nq) * (S // self.nq)
                else:
                    base = 0
            else:
                ntiles = self.NT
                base = 0
            for ti in range(ntiles):
                t0 = ti * T
                if first:
                    import os
                    bis = int(os.environ.get("BISECT", "99"))
                    self.load_x_tile(t0)
                    if bis == 1:
                        continue
                    if bis == 2:
                        self.norm_to_uT(0)
                        continue
                    self.ffn(0, 0, 0, 1)
                    if bis == 3:
                        continue
                    sc.dma("pool", self.h1T.rearrange("(c p) s -> p c s", p=128)[:, :, t0:t0 + T], self.hT[:], reads=hkeys,
                           writes=[("h1T", ti)])
                    self.norm_to_uT(2)
                    self.in_proj(0, t0)
                    continue
                if last and self.nq > 1:
                    col = bass.ds(base + t0, T)
                else:
                    col = slice(t0, t0 + T)
                hsrc = self.h1T.rearrange("(c p) s -> p c s", p=128)[:, :, col]
                ysrc = self.ycatT.rearrange("(c p) s -> p c s", p=128)[:, :, col]
                rkeys = [("h1T", i) for i in range(self.NT)] if last else [("h1T", ti)]
                ykeys = [("ycatT", i) for i in range(self.NT)] if last else [("ycatT", ti)]
                sc.dma("pool", self.hT[:], hsrc, reads=rkeys, writes=hkeys)
                sc.dma("pool", self.uT[:, 0:KM, :], ysrc, reads=ykeys, writes=["uT"])
                self.proj_to_fT(self.wo[layer], KM, self.uT, "uT")
                self.resid_update(layer * 6 + 3, False)
                self.ffn(layer, 1, layer * 6 + 4, layer * 6 + 5)
                if last:
                    self.store_out_tile(t0)
                else:
                    l2 = layer + 1
                    self.ffn(l2, 0, l2 * 6 + 0, l2 * 6 + 1)
                    sc.dma("pool", self.h1T.rearrange("(c p) s -> p c s", p=128)[:, :, t0:t0 + T], self.hT[:], reads=hkeys,
                           writes=[("h1T", ti)])
                    self.norm_to_uT(l2 * 6 + 2)
                    self.in_proj(l2, t0)
            for e in ("sp", "pool", "act", "dve", "pe"):
                sc.drain(e)

    def mixer_phase(self, l):
        which = self.mixers
        if "fox" in which:
            with ExitStack() as st:
                self.fox_phase(l, st)
                self.barrier()
        if "ret" in which:
            with ExitStack() as st:
                self.ret_phase(l, st)
                self.barrier()
        if "rwkv" in which:
            with ExitStack() as st:
                self.rwkv_phase(l, st)
                self.barrier()

    def small_stats(self, src, sqk, dim, eps, psb=7):
        nc, sc = self.nc, self.sc
        sc.op("act", lambda: nc.scalar.activation(out=self.msq[:], in_=src, func=AF.Square), reads=[sqk], writes=["msq"])
        sc.op("pe", lambda: nc.tensor.matmul(self.ps[psb][:], lhsT=self.ones_bf[:], rhs=self.msq[:], start=True, stop=True),
              reads=["msq", "ones_bf"], writes=["ps%d" % psb])
        sc.op("dve", lambda: nc.vector.tensor_scalar(out=self.mms[:], in0=self.ps[psb][:], scalar1=1.0 / dim, scalar2=eps,
                                                      op0=ALU.mult, op1=ALU.add), reads=["ps%d" % psb], writes=["mms"])
        sc.op("act", lambda: nc.scalar.activation(out=self.mms[:], in_=self.mms[:], func=AF.Sqrt), reads=["mms"], writes=["mms"])
        sc.op("dve", lambda: nc.vector.reciprocal(out=self.mrs[:], in_=self.mms[:]), reads=["mms"], writes=["mrs"])

    def fox_phase(self, l, st):
        nc, sc, S, I = self.nc, self.sc, self.S, self.I
        NB, NQT = S // 128, S // T
        SEG = min(2048, S)
        pT, vtok = self.pT, self.vtok
        fl = [self.sb(st, "fl%d" % i, [8, SEG], F32) for i in range(2)]
        cs = [self.sb(st, "cs%d" % i, [8, SEG], F32) for i in range(2)]
        one8 = self.sb(st, "one8", [8, SEG], F32)
        ccol = self.sb(st, "ccol", [128, NB, 8], F32)
        nfb = self.sb(st, "nfb", [8, 1], F32)
        gq = self.sb(st, "gq", [128, 2], F32)
        qn = self.sb(st, "qn", [128, S], BF16)
        kn = self.sb(st, "kn", [128, S], BF16)
        vv = self.sb(st, "vv", [128, NB, 128], BF16)
        masks = self.sb(st, "masks", [128, 4, T], F32)
        bq = [self.sb(st, "bq%d" % i, [128, T], F32) for i in range(2)]
        lg = [self.sb(st, "lg%d" % i, [128, T], F32) for i in range(2)]
        pp = [self.sb(st, "pp%d" % i, [128, T], BF16) for i in range(2)]
        x32 = [self.sb(st, "x32_%d" % i, [128, T], F32) for i in range(2)]
        self.msq = self.sb(st, "msq", [128, T], BF16)
        self.mms = self.sb(st, "mms", [128, T], F32)
        self.mrs = self.sb(st, "mrs", [128, T], F32)
        rd = self.sb(st, "rd", [128, T], F32)
        o32 = self.sb(st, "o32", [128, T], F32)
        yb = [self.sb(st, "yb%d" % i, [128, T], BF16) for i in range(2)]
        ps = self.ps
        sc.dma("pool", masks[:], I["cmix"][:, CM_MASK:CM_MASK + 4 * T].rearrange("p (j t) -> p j t", j=4), writes=["masks"])
        sc.op("pool", lambda: nc.gpsimd.memset(one8[:], 1.0), writes=["one8"])
        with nc.allow_non_contiguous_dma(reason="tiny per-head vectors"):
            sc.dma("pool", nfb[:], I["fox_f_bias"][l].rearrange("(p o) -> p o", o=1), writes=["nfb"])
            sc.dma("pool", gq[:], I["fox_qk_gain"][l].rearrange("j p -> p j"), writes=["gq"])
        sc.op("dve", lambda: nc.vector.tensor_scalar(out=nfb[:], in0=nfb[:], scalar1=-1.0, scalar2=None, op0=ALU.mult),
              reads=["nfb"], writes=["nfb"])
        sc.op("dve", lambda: nc.vector.tensor_scalar(out=gq[:, 0:1], in0=gq[:, 0:1], scalar1=float(128.0 ** -0.5), scalar2=None,
                                                      op0=ALU.mult), reads=["gq"], writes=["gq"])
        for sg in range(S // SEG):
            i = sg % 2
            c0 = sg * SEG
            sc.dma("pool", fl[i][:], pT[24 * 128:24 * 128 + 8, c0:c0 + SEG], reads=[("pT", 24, t) for t in range(NQT)],
                   writes=["fl%d" % i])
            sc.op("act", lambda: nc.scalar.activation(out=fl[i][:], in_=fl[i][:], func=AF.Exp, bias=nfb[:, 0:1], scale=-1.0),
                  reads=["fl%d" % i, "nfb"], writes=["fl%d" % i])
            sc.op("act", lambda: nc.scalar.activation(out=fl[i][:], in_=fl[i][:], func=AF.Ln, bias=self.one_col[0:8, 0:1]),
                  reads=["fl%d" % i], writes=["fl%d" % i])
            init = 0.0 if sg == 0 else cs[1 - i][:, SEG - 1:SEG]
            sc.op("dve", lambda: nc.vector.tensor_tensor_scan(out=cs[i][:], data0=one8[:], data1=fl[i][:], initial=init,
                                                               op0=ALU.mult, op1=ALU.add),
                  reads=["fl%d" % i, "one8", "cs%d" % (1 - i)], writes=["cs%d" % i])
            sc.op("dve", lambda: nc.vector.tensor_scalar(out=fl[i][:], in0=cs[i][:], scalar1=-1.0, scalar2=None, op0=ALU.mult),
                  reads=["cs%d" % i], writes=["fl%d" % i])
            sc.dma("pool", self.cumT[:, c0:c0 + SEG], fl[i][:], reads=["fl%d" % i], writes=["cumT"])
            nb = SEG // 128
            for b in range(nb):
                sc.op("pe", lambda: nc.tensor.transpose(out=ps[7][:, b * 8:(b + 1) * 8], in_=cs[i][0:8, b * 128:(b + 1) * 128],
                                                         identity=self.ident[0:8, 0:8]),
                      reads=["cs%d" % i, "cst"], writes=["ps7"])
            sc.op("dve", lambda: nc.vector.tensor_copy(out=ccol[:, sg * nb:(sg + 1) * nb, :],
                                                        in_=ps[7][:, 0:nb * 8].rearrange("p (b e) -> p b e", e=8)),
                  reads=["ps7"], writes=["ccol"])
        for h in range(8):
            for ti in range(NQT):
                t0 = ti * T
                for (chunk, dst, dk, gi) in ((h, qn, "qn", 0), (8 + h, kn, "kn", 1)):
                    i = (2 * ti + gi) % 2
                    sc.dma("pool", x32[i][:], pT[chunk * 128:(chunk + 1) * 128, t0:t0 + T], reads=[("pT", chunk, ti)],
                           writes=["x32_%d" % i])
                    self.small_stats(x32[i][:], "x32_%d" % i, 128.0, EPS)
                    sc.op("dve", lambda: nc.vector.scalar_tensor_tensor(out=dst[:, t0:t0 + T], in0=x32[i][:], scalar=gq[:, gi:gi + 1],
                                                                         in1=self.mrs[:], op0=ALU.mult, op1=ALU.mult),
                          reads=["x32_%d" % i, "gq", "mrs"], writes=[dk])
            nsp = max(1, NB // 32)
            bp = NB // nsp
            for j in range(nsp):
                sc.dma("pool", vv[:, j * bp:(j + 1) * bp, :],
                       vtok[j * bp * 128:(j + 1) * bp * 128, h * 128:(h + 1) * 128].rearrange("(b p) d -> p b d", p=128),
                       reads=[("vtok", t) for t in range(NQT)], writes=["vv"])
            for ti in range(NQT):
                t0 = ti * T
                i = ti % 2
                sc.dma("pool", bq[i][:], self.cumT[h:h + 1, t0:t0 + T].partition_broadcast(128), reads=["cumT"], writes=["bq%d" % i])
                nkb = 4 * (ti + 1)
                po, pd = 2 + i, 4 + i
                for kb in range(nkb):
                    j = kb % 2
                    sc.op("pe", lambda: nc.tensor.matmul(ps[j][:], lhsT=kn[:, kb * 128:(kb + 1) * 128], rhs=qn[:, t0:t0 + T],
                                                          start=True, stop=True), reads=["kn", "qn"], writes=["ps%d" % j])
                    sc.op("dve", lambda: nc.vector.tensor_tensor(out=lg[j][:], in0=ps[j][:], in1=bq[i][:], op=ALU.add),
                          reads=["ps%d" % j, "bq%d" % i], writes=["lg%d" % j])
                    if kb >= 4 * ti:
                        sc.op("pool", lambda: nc.gpsimd.tensor_tensor(out=lg[j][:], in0=lg[j][:], in1=masks[:, kb - 4 * ti, :],
                                                                      op=ALU.add), reads=["lg%d" % j, "masks"], writes=["lg%d" % j])
                    sc.op("act", lambda: nc.scalar.activation(out=pp[j][:], in_=lg[j][:], func=AF.Exp, bias=ccol[:, kb, h:h + 1]),
                          reads=["lg%d" % j, "ccol"], writes=["pp%d" % j])
                    sc.op("pe", lambda: nc.tensor.matmul(ps[po][:], lhsT=vv[:, kb, :], rhs=pp[j][:], start=(kb == 0),
                                                          stop=(kb == nkb - 1)), reads=["vv", "pp%d" % j], writes=["ps%d" % po])
                    sc.op("pe", lambda: nc.tensor.matmul(ps[pd][:], lhsT=self.ones_bf[:], rhs=pp[j][:], start=(kb == 0),
                                                          stop=(kb == nkb - 1)), reads=["ones_bf", "pp%d" % j], writes=["ps%d" % pd])
                sc.op("dve", lambda: nc.vector.reciprocal(out=rd[:], in_=ps[pd][:]), reads=["ps%d" % pd], writes=["rd"])
                sc.op("dve", lambda: nc.vector.tensor_tensor(out=o32[:], in0=ps[po][:], in1=rd[:], op=ALU.mult),
                      reads=["ps%d" % po, "rd"], writes=["o32"])
                gi_ = ti % 2
                sc.dma("pool", x32[gi_][:], pT[(16 + h) * 128:(17 + h) * 128, t0:t0 + T], reads=[("pT", 16 + h, ti)],
                       writes=["x32_%d" % gi_])
                sc.op("act", lambda: nc.scalar.activation(out=x32[gi_][:], in_=x32[gi_][:], func=AF.Sigmoid),
                      reads=["x32_%d" % gi_], writes=["x32_%d" % gi_])
                sc.op("dve", lambda: nc.vector.tensor_tensor(out=yb[i][:], in0=o32[:], in1=x32[gi_][:], op=ALU.mult),
                      reads=["o32", "x32_%d" % gi_], writes=["yb%d" % i])
                sc.dma("pool", self.ycatT[h * 128:(h + 1) * 128, t0:t0 + T], yb[i][:], reads=["yb%d" % i], writes=[("ycatT", ti)])

    def ret_phase(self, l, st):
        nc, sc, S, I = self.nc, self.sc, self.S, self.I
        NQT = S // T
        pT, vtok = self.pT, self.vtok
        ps = self.ps
        rm = self.sb(st, "rm", [128, 128], F32)
        din = self.sb(st, "din", [128, 4, 128], F32)
        qdec = self.sb(st, "qdec", [128, 4, T], F32)
        kdec = self.sb(st, "kdec", [128, 4], F32)
        q32 = self.sb(st, "q32", [128, T], F32)
        k32 = self.sb(st, "k32", [128, T], F32)
        cst_ = self.sb(st, "rcos", [128, T], F32)
        snt = self.sb(st, "rsin", [128, T], F32)
        t1 = self.sb(st, "rt1", [128, T], F32)
        t2 = self.sb(st, "rt2", [128, T], F32)
        qr = self.sb(st, "qr", [128, T], F32)
        kr = self.sb(st, "kr", [128, T], F32)
        qd = self.sb(st, "qd", [128, T], BF16)
        vt = self.sb(st, "rvt", [128, 4, 128], BF16)
        PT = [self.sb(st, "rPT%d" % i, [128, 128], BF16) for i in range(2)]
        kd = [self.sb(st, "rkd%d" % i, [128, 128], BF16) for i in range(2)]
        st32 = [self.sb(st, "rst32_%d" % h, [128, 128], F32) for h in range(4)]
        stbf = [self.sb(st, "rstbf_%d" % h, [128, 128], BF16) for h in range(4)]
        o32 = self.sb(st, "ro32", [128, T], F32)
        g32 = self.sb(st, "rg32", [128, T], F32)
        yb = [self.sb(st, "ryb%d" % i, [128, T], BF16) for i in range(2)]
        self.msq = self.sb(st, "msq_r", [128, T], BF16)
        self.mms = self.sb(st, "mms_r", [128, T], F32)
        self.mrs = self.sb(st, "mrs_r", [128, T], F32)
        cm = I["cmix"]
        sc.dma("pool", rm[:], cm[:, CM_RM:CM_RM + 128], writes=["rm"])
        sc.dma("pool", din[:], cm[:, CM_DIN:CM_DIN + 512].rearrange("p (h i) -> p h i", h=4), writes=["din"])
        sc.dma("pool", qdec[:], cm[:, CM_QDEC:CM_QDEC + 4 * T].rearrange("p (h i) -> p h i", h=4), writes=["qdec"])
        sc.dma("pool", kdec[:], cm[:, CM_KDEC:CM_KDEC + 4], writes=["kdec"])
        for h in range(4):
            sc.op("pool", lambda: nc.gpsimd.memset(st32[h][:], 0.0), writes=["rst32_%d" % h])
            sc.op("pool", lambda: nc.gpsimd.memset(stbf[h][:], 0.0), writes=["rstbf_%d" % h])
        for ti in range(NQT):
            t0 = ti * T
            sc.dma("pool", cst_[:], I["rot"][0, :, t0:t0 + T], writes=["rcos"])
            sc.dma("pool", snt[:], I["rot"][1, :, t0:t0 + T], writes=["rsin"])
            for h in range(4):
                gam = RET_GAMMA[h]
                sc.dma("pool", q32[:], pT[(25 + h) * 128:(26 + h) * 128, t0:t0 + T], reads=[("pT", 25 + h, ti)], writes=["q32"])
                sc.dma("pool", k32[:], pT[(29 + h) * 128:(30 + h) * 128, t0:t0 + T], reads=[("pT", 29 + h, ti)], writes=["k32"])
                sc.dma("pool", vt[:], vtok[t0:t0 + T, 1024 + h * 128:1024 + (h + 1) * 128].rearrange("(c p) e -> p c e", p=128),
                       reads=[("vtok", ti)], writes=["rvt"])
                for (src, sk, dst, dk) in ((q32, "q32", qr, "qr"), (k32, "k32", kr, "kr")):
                    sc.op("pe", lambda: nc.tensor.matmul(ps[0][:], lhsT=rm[:], rhs=src[:], start=True, stop=True),
                          reads=["rm", sk], writes=["ps0"])
                    sc.op("dve", lambda: nc.vector.tensor_tensor(out=t1[:], in0=src[:], in1=cst_[:], op=ALU.mult),
                          reads=[sk, "rcos"], writes=["rt1"])
                    sc.op("dve", lambda: nc.vector.tensor_tensor(out=t2[:], in0=ps[0][:], in1=snt[:], op=ALU.mult),
                          reads=["ps0", "rsin"], writes=["rt2"])
                    sc.op("pool", lambda: nc.gpsimd.tensor_tensor(out=dst[:], in0=t1[:], in1=t2[:], op=ALU.add),
                          reads=["rt1", "rt2"], writes=[dk])
                sc.op("dve", lambda: nc.vector.tensor_tensor(out=qd[:], in0=qr[:], in1=qdec[:, h, :], op=ALU.mult),
                      reads=["qr", "qdec"], writes=["qd"])
                for c in range(T // 128):
                    cs_ = slice(c * 128, (c + 1) * 128)
                    j = c % 2
                    sc.op("pe", lambda: nc.tensor.matmul(ps[1][:, 0:128], lhsT=kr[:, cs_], rhs=qr[:, cs_], start=True, stop=True),
                          reads=["kr", "qr"], writes=["ps1"])
                    sc.op("dve", lambda: nc.vector.tensor_tensor(out=PT[j][:], in0=ps[1][:, 0:128], in1=din[:, h, :], op=ALU.mult),
                          reads=["ps1", "din"], writes=["rPT%d" % j])
                    sc.op("pe", lambda: nc.tensor.matmul(ps[2][:, cs_], lhsT=vt[:, c, :], rhs=PT[j][:], start=True, stop=False),
                          reads=["rvt", "rPT%d" % j], writes=["ps2"])
                    sc.op("pe", lambda: nc.tensor.matmul(ps[2][:, cs_], lhsT=stbf[h][:], rhs=qd[:, cs_], start=False, stop=True),
                          reads=["rstbf_%d" % h, "qd"], writes=["ps2"])
                    sc.op("pe", lambda: nc.tensor.transpose(out=ps[3][:, 0:128], in_=kr[:, cs_], identity=self.ident),
                          reads=["kr", "cst"], writes=["ps3"])
                    sc.op("act", lambda: nc.scalar.activation(out=kd[j][:], in_=ps[3][:, 0:128], func=AF.Copy, scale=kdec[:, h:h + 1]),
                          reads=["ps3", "kdec"], writes=["rkd%d" % j])
                    sc.op("pe", lambda: nc.tensor.matmul(ps[4][:, 0:128], lhsT=kd[j][:], rhs=vt[:, c, :], start=True, stop=True),
                          reads=["rkd%d" % j, "rvt"], writes=["ps4"])
                    sc.op("dve", lambda: nc.vector.scalar_tensor_tensor(out=st32[h][:], in0=st32[h][:], scalar=float(gam ** 128),
                                                                         in1=ps[4][:, 0:128], op0=ALU.mult, op1=ALU.add),
                          reads=["ps4", "rst32_%d" % h], writes=["rst32_%d" % h])
                    sc.op("act", lambda: nc.scalar.copy(out=stbf[h][:], in_=st32[h][:]), reads=["rst32_%d" % h],
                          writes=["rstbf_%d" % h])
                sc.op("dve", lambda: nc.vector.tensor_copy(out=o32[:], in_=ps[2][:]), reads=["ps2"], writes=["ro32"])
                self.small_stats(o32[:], "ro32", 128.0, EPS)
                sc.dma("pool", g32[:], pT[(33 + h) * 128:(34 + h) * 128, t0:t0 + T], reads=[("pT", 33 + h, ti)], writes=["rg32"])
                sc.op("act", lambda: nc.scalar.activation(out=g32[:], in_=g32[:], func=AF.Silu), reads=["rg32"], writes=["rg32"])
                sc.op("dve", lambda: nc.vector.tensor_tensor(out=o32[:], in0=o32[:], in1=self.mrs[:], op=ALU.mult),
                      reads=["ro32", "mrs"], writes=["ro32"])
                i = (ti * 4 + h) % 2
                sc.op("dve", lambda: nc.vector.tensor_tensor(out=yb[i][:], in0=o32[:], in1=g32[:], op=ALU.mult),
                      reads=["ro32", "rg32"], writes=["ryb%d" % i])
                sc.dma("pool", self.ycatT[1024 + h * 128:1024 + (h + 1) * 128, t0:t0 + T], yb[i][:], reads=["ryb%d" % i],
                       writes=[("ycatT", ti)])


    def rwkv_phase(self, l, st):
        nc, sc, S, I = self.nc, self.sc, self.S, self.I
        NQT = S // T
        NC8 = T // 64
        pT, ps = self.pT, self.ps
        cm = I["cmix"]
        V = nc.vector
        def sbt(name, shape, dt=F32):
            return self.sb(st, "w_" + name, shape, dt)
        def dve(fn, reads, writes):
            return sc.op("dve", fn, reads, writes)
        def act(fn, reads, writes):
            return sc.op("act", fn, reads, writes)
        def pe(fn, reads, writes):
            return sc.op("pe", fn, reads, writes)
        msk2 = sbt("msk2", [64, NC8, 128]); mskT = sbt("mskT", [64, NC8, 64]); I8 = sbt("I8", [64, NC8, 64])
        scanm = sbt("scanm", [64, T]); ones64 = sbt("ones64", [64, 64])
        sc.dma("pool", msk2[:], cm[0:64, CM_MSK2:CM_MSK2 + NC8 * 128].rearrange("p (c t) -> p c t", c=NC8), writes=["msk2"])
        sc.dma("pool", mskT[:], cm[0:64, CM_MSKT:CM_MSKT + NC8 * 64].rearrange("p (c t) -> p c t", c=NC8), writes=["mskT"])
        sc.dma("pool", I8[:], cm[0:64, CM_I8:CM_I8 + NC8 * 64].rearrange("p (c t) -> p c t", c=NC8), writes=["I8"])
        sc.dma("pool", scanm[:], cm[0:64, CM_SCAN:CM_SCAN + T], writes=["scanm"])
        sc.op("pool", lambda: nc.gpsimd.memset(ones64[:], 1.0), writes=["ones64"])
        pv = sbt("pv", [64, 6, 8]); pmu = sbt("pmu", [64, 3, 8]); prk = sbt("prk", [64, 8]); pv0 = sbt("pv0", [64, 8])
        omka = sbt("omka", [64, 8])
        muw = sbt("muw", [96, 1]); mua = sbt("mua", [96, 1]); mug = sbt("mug", [128, 2])
        w2s = sbt("w2s", [96, 512]); a2s = sbt("a2s", [96, 512]); g2s = sbt("g2s", [128, 2, 512]); v2s = sbt("v2s", [64, 512])
        mu = I["rwkv_mu"][l]
        with nc.allow_non_contiguous_dma(reason="tiny per-channel parameter vectors"):
            for j in range(6):
                sc.dma("pool", pv[:, j, :], I["rwkv_vec"][l, j].rearrange("(h p) -> p h", p=64), writes=["pv"])
            for j in range(3):
                sc.dma("pool", pmu[:, j, :], mu[j * 512:(j + 1) * 512].rearrange("(h p) -> p h", p=64), writes=["pmu"])
            sc.dma("pool", prk[:], I["rwkv_r_k"][l].rearrange("h p -> p h"), writes=["prk"])
            sc.dma("pool", muw[:], mu[1536:1632].rearrange("(p o) -> p o", o=1), writes=["muw"])
            sc.dma("pool", mua[:], mu[1632:1728].rearrange("(p o) -> p o", o=1), writes=["mua"])
            sc.dma("pool", mug[:], mu[1728:1984].rearrange("(c p) -> p c", p=128), writes=["mug"])
            if l > 0:
                sc.dma("pool", pv0[:], I["rwkv_v0"][l - 1].rearrange("(h p) -> p h", p=64), writes=["pv0"])
        sc.dma("pool", w2s[:], I["rwkv_w2"][l], writes=["w2s"])
        sc.dma("pool", a2s[:], I["rwkv_a2"][l], writes=["a2s"])
        sc.dma("pool", g2s[:], I["rwkv_g2"][l].rearrange("(c p) n -> p c n", p=128), writes=["g2s"])
        if l > 0:
            sc.dma("pool", v2s[:], I["rwkv_v2"][l - 1], writes=["v2s"])
        dve(lambda: V.tensor_scalar(out=omka[:], in0=pv[:, 3, :], scalar1=-1.0, scalar2=1.0, op0=ALU.mult, op1=ALU.add),
            ["pv"], ["omka"])
        STs = [sbt("ST%d" % h, [64, 64]) for h in range(8)]
        for h in range(8):
            sc.op("pool", lambda: nc.gpsimd.memset(STs[h][:], 0.0), writes=["ST%d" % h])
        raw = [sbt("raw%d" % i, [128, T + 1]) for i in range(2)]
        dtmp = sbt("dtmp", [128, T])
        zwl = sbt("zwl", [96, T]); zal = sbt("zal", [96, T]); zgl = sbt("zgl", [128, 2, T]); pvr = sbt("pvr", [64, T])
        names = ["r", "k", "v", "ld", "a", "g", "kk", "kkn", "k2", "b", "tt", "Lc", "Lx", "Ep", "Em", "Ex", "kt", "bt", "vf", "bon", "y",
                 "ysq", "mean", "msq2", "rs"]
        W = {n: sbt(n, [64, T]) for n in names}
        AR = sbt("AR", [64, NC8, 128]); NA = sbt("NA", [64, NC8, 128]); KA = sbt("KA", [64, NC8, 128])
        NT = sbt("NT", [64, NC8, 64]); Mx = sbt("Mx", [64, NC8, 64])
        Nb = [sbt("Nb%d" % i, [64, NC8, 64]) for i in range(2)]
        NTb = [sbt("NTb%d" % i, [64, NC8, 64]) for i in range(2)]
        vtk = sbt("vtk", [64, NC8, 64]); ktk = sbt("ktk", [64, NC8, 64]); btk = sbt("btk", [64, NC8, 64])
        W0 = sbt("W0", [64, 64]); UT = sbt("UT", [64, 64])
        yb = [sbt("yb%d" % i, [64, T], BF16) for i in range(2)]
        id64 = self.ident[0:64, 0:64]
        c3 = lambda ap: ap.rearrange("p (c t) -> p c t", t=64)
        rawrr = [0]

        def load_shift(row0, rows, chunk, ti, mucol, dst, dkey):
            t0 = ti * T
            i = rawrr[0] % 2
            rawrr[0] += 1
            rk = "w_raw%d" % i
            rd_keys = [("pT", chunk, ti)] + ([("pT", chunk, ti - 1)] if ti > 0 else [])
            if ti == 0:
                sc.op("pool", lambda: nc.gpsimd.memset(raw[i][0:rows, 0:1], 0.0), writes=[rk])
                sc.dma("pool", raw[i][0:rows, 1:T + 1], pT[row0:row0 + rows, 0:T], reads=rd_keys, writes=[rk])
            else:
                sc.dma("pool", raw[i][0:rows, :], pT[row0:row0 + rows, t0 - 1:t0 + T], reads=rd_keys, writes=[rk])
            dve(lambda: V.tensor_tensor(out=dtmp[0:rows, :], in0=raw[i][0:rows, 0:T], in1=raw[i][0:rows, 1:T + 1], op=ALU.subtract),
                [rk], ["dtmp"])
            dve(lambda: V.scalar_tensor_tensor(out=dst, in0=dtmp[0:rows, :], scalar=mucol, in1=raw[i][0:rows, 1:T + 1],
                                               op0=ALU.mult, op1=ALU.add), ["dtmp", rk, "pmu", "muw", "mua", "mug"], [dkey])

        for ti in range(NQT):
            t0 = ti * T
            load_shift(49 * 128, 96, 49, ti, muw[:, 0:1], zwl[:], "zwl")
            act(lambda: nc.scalar.activation(out=zwl[:], in_=zwl[:], func=AF.Tanh), ["zwl"], ["zwl"])
            load_shift(50 * 128, 96, 50, ti, mua[:, 0:1], zal[:], "zal")
            for gc in range(2):
                load_shift((51 + gc) * 128, 128, 51 + gc, ti, mug[:, gc:gc + 1], zgl[:, gc, :], "zgl")
            act(lambda: nc.scalar.activation(out=zgl[:], in_=zgl[:], func=AF.Sigmoid), ["zgl"], ["zgl"])
            if l > 0:
                sc.dma("pool", pvr[:], pT[53 * 128:53 * 128 + 64, t0:t0 + T], reads=[("pT", 53, ti)], writes=["pvr"])
            for h in range(8):
                hc = slice(h * 64, (h + 1) * 64)
                ro = (h % 2) * 64
                vec = lambda j: pv[:, j, h:h + 1]
                r, k, v, ld, a, g = W["r"], W["k"], W["v"], W["ld"], W["a"], W["g"]
                load_shift((37 + h // 2) * 128 + ro, 64, 37 + h // 2, ti, pmu[:, 0, h:h + 1], r[:], "r")
                load_shift((41 + h // 2) * 128 + ro, 64, 41 + h // 2, ti, pmu[:, 1, h:h + 1], k[:], "k")
                load_shift((45 + h // 2) * 128 + ro, 64, 45 + h // 2, ti, pmu[:, 2, h:h + 1], v[:], "v")
                if l == 0:
                    sc.dma("pool", self.vfirstT[hc, t0:t0 + T], v[:], reads=["v"], writes=[("vfirst", h, ti)])
                else:
                    pe(lambda: nc.tensor.matmul(ps[7][0:64, :], lhsT=v2s[:, hc], rhs=pvr[:], start=True, stop=True), ["v2s", "pvr"], ["ps7"])
                    act(lambda: nc.scalar.activation(out=W["tt"][:], in_=ps[7][0:64, :], func=AF.Sigmoid, bias=pv0[:, h:h + 1]),
                        ["ps7", "pv0"], ["tt"])
                    sc.dma("pool", W["vf"][:], self.vfirstT[hc, t0:t0 + T], reads=[("vfirst", h, ti)], writes=["vf"])
                    dve(lambda: V.tensor_tensor(out=W["vf"][:], in0=W["vf"][:], in1=v[:], op=ALU.subtract), ["vf", "v"], ["vf"])
                    dve(lambda: V.tensor_tensor(out=W["vf"][:], in0=W["vf"][:], in1=W["tt"][:], op=ALU.mult), ["vf", "tt"], ["vf"])
                    dve(lambda: V.tensor_tensor(out=v[:], in0=v[:], in1=W["vf"][:], op=ALU.add), ["vf", "v"], ["v"])
                pe(lambda: nc.tensor.matmul(ps[7][0:64, :], lhsT=w2s[:, hc], rhs=zwl[:], start=True, stop=True), ["w2s", "zwl"], ["ps7"])
                act(lambda: nc.scalar.activation(out=ld[:], in_=ps[7][0:64, :], func=AF.Sigmoid, bias=vec(0)), ["ps7", "pv"], ["ld"])
                dve(lambda: V.tensor_scalar(out=ld[:], in0=ld[:], scalar1=-0.6065306597126334, scalar2=None, op0=ALU.mult), ["ld"], ["ld"])
                pe(lambda: nc.tensor.matmul(ps[7][0:64, :], lhsT=a2s[:, hc], rhs=zal[:], start=True, stop=True), ["a2s", "zal"], ["ps7"])
                act(lambda: nc.scalar.activation(out=a[:], in_=ps[7][0:64, :], func=AF.Sigmoid, bias=vec(1)), ["ps7", "pv"], ["a"])
                for gc in range(2):
                    pe(lambda: nc.tensor.matmul(ps[7][0:64, :], lhsT=g2s[:, gc, hc], rhs=zgl[:, gc, :], start=(gc == 0), stop=(gc == 1)),
                       ["g2s", "zgl"], ["ps7"])
                act(lambda: nc.scalar.copy(out=g[:], in_=ps[7][0:64, :]), ["ps7"], ["g"])
                kk, kkn, k2, b, tt = W["kk"], W["kkn"], W["k2"], W["b"], W["tt"]
                dve(lambda: V.tensor_scalar(out=kk[:], in0=k[:], scalar1=vec(2), scalar2=None, op0=ALU.mult), ["k", "pv"], ["kk"])
                dve(lambda: V.tensor_tensor(out=tt[:], in0=kk[:], in1=kk[:], op=ALU.mult), ["kk"], ["tt"])
                pe(lambda: nc.tensor.matmul(ps[7][0:64, :], lhsT=ones64[:], rhs=tt[:], start=True, stop=True), ["ones64", "tt"], ["ps7"])
                act(lambda: nc.scalar.activation(out=tt[:], in_=ps[7][0:64, :], func=AF.Sqrt), ["ps7"], ["tt"])
                dve(lambda: V.tensor_scalar(out=tt[:], in0=tt[:], scalar1=1e-12, scalar2=None, op0=ALU.max), ["tt"], ["tt"])
                dve(lambda: V.reciprocal(out=tt[:], in_=tt[:]), ["tt"], ["tt"])
                dve(lambda: V.tensor_tensor(out=kkn[:], in0=kk[:], in1=tt[:], op=ALU.mult), ["kk", "tt"], ["kkn"])
                dve(lambda: V.tensor_scalar(out=tt[:], in0=a[:], scalar1=vec(3), scalar2=omka[:, h:h + 1], op0=ALU.mult, op1=ALU.add),
                    ["a", "pv", "omka"], ["tt"])
                dve(lambda: V.tensor_tensor(out=k2[:], in0=k[:], in1=tt[:], op=ALU.mult), ["k", "tt"], ["k2"])
                dve(lambda: V.tensor_tensor(out=b[:], in0=kkn[:], in1=a[:], op=ALU.mult), ["kkn", "a"], ["b"])
                dve(lambda: V.scalar_tensor_tensor(out=tt[:], in0=r[:], scalar=prk[:, h:h + 1], in1=k2[:], op0=ALU.mult, op1=ALU.mult),
                    ["r", "k2", "prk"], ["tt"])
                pe(lambda: nc.tensor.matmul(ps[7][0:64, :], lhsT=ones64[:], rhs=tt[:], start=True, stop=True), ["ones64", "tt"], ["ps7"])
                dve(lambda: V.tensor_tensor(out=W["bon"][:], in0=ps[7][0:64, :], in1=v[:], op=ALU.mult), ["ps7", "v"], ["bon"])
                Lc, Lx, Ep, Em, Ex, kt, bt = W["Lc"], W["Lx"], W["Ep"], W["Em"], W["Ex"], W["kt"], W["bt"]
                dve(lambda: V.tensor_tensor_scan(out=Lc[:], data0=scanm[:], data1=ld[:], initial=0.0, op0=ALU.mult, op1=ALU.add),
                    ["scanm", "ld"], ["Lc"])
                dve(lambda: V.tensor_tensor(out=Lx[:], in0=Lc[:], in1=ld[:], op=ALU.subtract), ["Lc", "ld"], ["Lx"])
                act(lambda: nc.scalar.activation(out=Ep[:], in_=Lc[:], func=AF.Exp), ["Lc"], ["Ep"])
                act(lambda: nc.scalar.activation(out=Em[:], in_=Lc[:], func=AF.Exp, scale=-1.0), ["Lc"], ["Em"])
                act(lambda: nc.scalar.activation(out=Ex[:], in_=Lx[:], func=AF.Exp), ["Lx"], ["Ex"])
                dve(lambda: V.scalar_tensor_tensor(out=AR[:, :, 0:64], in0=c3(kkn[:]), scalar=-1.0, in1=c3(Ex[:]), op0=ALU.mult, op1=ALU.mult),
                    ["kkn", "Ex"], ["AR"])
                dve(lambda: V.tensor_tensor(out=AR[:, :, 64:128], in0=c3(r[:]), in1=c3(Ep[:]), op=ALU.mult), ["r", "Ep"], ["AR"])
                dve(lambda: V.tensor_tensor(out=kt[:], in0=k2[:], in1=Em[:], op=ALU.mult), ["k2", "Em"], ["kt"])
                dve(lambda: V.tensor_tensor(out=bt[:], in0=b[:], in1=Em[:], op=ALU.mult), ["b", "Em"], ["bt"])
                for c in range(NC8):
                    cc = slice(c * 64, (c + 1) * 64)
                    bk, co = c // 4, (c % 4) * 128
                    pe(lambda: nc.tensor.matmul(ps[bk][0:64, co:co + 128], lhsT=bt[:, cc], rhs=AR[:, c, :], start=True, stop=True),
                       ["bt", "AR"], ["ps%d" % bk])
                    pe(lambda: nc.tensor.matmul(ps[2 + bk][0:64, co:co + 128], lhsT=kt[:, cc], rhs=AR[:, c, :], start=True, stop=True),
                       ["kt", "AR"], ["ps%d" % (2 + bk)])
                    pe(lambda: nc.tensor.matmul(ps[4][0:64, cc], lhsT=AR[:, c, 0:64], rhs=bt[:, cc], start=True, stop=True),
                       ["bt", "AR"], ["ps4"])
                for bk in range(2):
                    cs_ = slice(bk * 4, bk * 4 + 4)
                    dve(lambda: V.tensor_tensor(out=NA[:, cs_, :], in0=ps[bk][0:64, :].rearrange("p (c t) -> p c t", t=128),
                                                in1=msk2[:, cs_, :], op=ALU.mult), ["ps%d" % bk, "msk2"], ["NA"])
                    dve(lambda: V.tensor_tensor(out=KA[:, cs_, :], in0=ps[2 + bk][0:64, :].rearrange("p (c t) -> p c t", t=128),
                                                in1=msk2[:, cs_, :], op=ALU.mult), ["ps%d" % (2 + bk), "msk2"], ["KA"])
                dve(lambda: V.tensor_tensor(out=NT[:], in0=c3(ps[4][0:64, :]), in1=mskT[:], op=ALU.mult), ["ps4", "mskT"], ["NT"])
                dve(lambda: V.tensor_tensor(out=Mx[:], in0=NA[:, :, 0:64], in1=I8[:], op=ALU.add), ["NA", "I8"], ["Mx"])
                curN, curNT, kN, kNT = NA[:, :, 0:64], NT[:], "NA", "NT"
                for rnd in range(5):
                    i = rnd % 2
                    lastr = (rnd == 4)
                    for c in range(NC8):
                        cc = slice(c * 64, (c + 1) * 64)
                        if not lastr:
                            pe(lambda: nc.tensor.matmul(ps[5][0:64, cc], lhsT=curNT[:, c, :], rhs=curN[:, c, :], start=True, stop=True),
                               [kN, kNT], ["ps5"])
                        pe(lambda: nc.tensor.matmul(ps[6][0:64, cc], lhsT=curN[:, c, :], rhs=curNT[:, c, :], start=True, stop=True),
                           [kN, kNT], ["ps6"])
                    if not lastr:
                        act(lambda: nc.scalar.copy(out=Nb[i][:], in_=c3(ps[5][0:64, :])), ["ps5"], ["Nb%d" % i])
                    dve(lambda: V.tensor_copy(out=NTb[i][:], in_=c3(ps[6][0:64, :])), ["ps6"], ["NTb%d" % i])
                    for c in range(NC8):
                        cc = slice(c * 64, (c + 1) * 64)
                        pe(lambda: nc.tensor.matmul(ps[7][0:64, cc], lhsT=NTb[i][:, c, :], rhs=Mx[:, c, :], start=True, stop=True),
                           ["NTb%d" % i, "Mx"], ["ps7"])
                    dve(lambda: V.tensor_tensor(out=Mx[:], in0=Mx[:], in1=c3(ps[7][0:64, :]), op=ALU.add), ["ps7", "Mx"], ["Mx"])
                    curN, curNT, kN, kNT = Nb[i][:], NTb[i][:], "Nb%d" % i, "NTb%d" % i
                for (src, sk, dst, dk, bk) in ((v, "v", vtk, "vtk", 0), (kt, "kt", ktk, "ktk", 1), (bt, "bt", btk, "btk", 2)):
                    for c in range(NC8):
                        cc = slice(c * 64, (c + 1) * 64)
                        pe(lambda: nc.tensor.transpose(out=ps[bk][0:64, cc], in_=src[:, cc], identity=id64), [sk, "cst"], ["ps%d" % bk])
                    act(lambda: nc.scalar.copy(out=dst[:], in_=c3(ps[bk][0:64, :])), ["ps%d" % bk], [dk])
                ST, sk_ = STs[h], "ST%d" % h
                for c in range(NC8):
                    cc = slice(c * 64, (c + 1) * 64)
                    pe(lambda: nc.tensor.matmul(ps[3][0:64, 0:64], lhsT=AR[:, c, 0:64], rhs=ST[:], start=True, stop=False), ["AR", sk_], ["ps3"])
                    pe(lambda: nc.tensor.matmul(ps[3][0:64, 0:64], lhsT=KA[:, c, 0:64], rhs=vtk[:, c, :], start=False, stop=True),
                       ["KA", "vtk"], ["ps3"])
                    act(lambda: nc.scalar.copy(out=W0[:], in_=ps[3][0:64, 0:64]), ["ps3"], ["W0"])
                    pe(lambda: nc.tensor.matmul(ps[4][0:64, 0:64], lhsT=Mx[:, c, :], rhs=W0[:], start=True, stop=True), ["Mx", "W0"], ["ps4"])
                    act(lambda: nc.scalar.copy(out=UT[:], in_=ps[4][0:64, 0:64]), ["ps4"], ["UT"])
                    pe(lambda: nc.tensor.matmul(ps[5][0:64, cc], lhsT=ST[:], rhs=AR[:, c, 64:128], start=True, stop=False), [sk_, "AR"], ["ps5"])
                    pe(lambda: nc.tensor.matmul(ps[5][0:64, cc], lhsT=UT[:], rhs=NA[:, c, 64:128], start=False, stop=False), ["UT", "NA"], ["ps5"])
                    pe(lambda: nc.tensor.matmul(ps[5][0:64, cc], lhsT=vtk[:, c, :], rhs=KA[:, c, 64:128], start=False, stop=True),
                       ["vtk", "KA"], ["ps5"])
                    pe(lambda: nc.tensor.matmul(ps[6][0:64, 0:64], lhsT=btk[:, c, :], rhs=UT[:], start=True, stop=False), ["btk", "UT"], ["ps6"])
                    pe(lambda: nc.tensor.matmul(ps[6][0:64, 0:64], lhsT=ktk[:, c, :], rhs=vtk[:, c, :], start=False, stop=True),
                       ["ktk", "vtk"], ["ps6"])
                    dve(lambda: V.tensor_tensor(out=ST[:], in0=ST[:], in1=ps[6][0:64, 0:64], op=ALU.add), [sk_, "ps6"], [sk_])
                    dve(lambda: V.tensor_scalar(out=ST[:], in0=ST[:], scalar1=Ep[:, c * 64 + 63:c * 64 + 64], scalar2=None, op0=ALU.mult),
                        [sk_, "Ep"], [sk_])
                y, ysq, mean, msq2, rs = W["y"], W["ysq"], W["mean"], W["msq2"], W["rs"]
                act(lambda: nc.scalar.copy(out=y[:], in_=ps[5][0:64, :]), ["ps5"], ["y"])
                pe(lambda: nc.tensor.matmul(ps[7][0:64, :], lhsT=ones64[:], rhs=y[:], start=True, stop=True), ["ones64", "y"], ["ps7"])
                dve(lambda: V.tensor_tensor(out=ysq[:], in0=y[:], in1=y[:], op=ALU.mult), ["y"], ["ysq"])
                pe(lambda: nc.tensor.matmul(ps[0][0:64, :], lhsT=ones64[:], rhs=ysq[:], start=True, stop=True), ["ones64", "ysq"], ["ps0"])
                dve(lambda: V.tensor_scalar(out=mean[:], in0=ps[7][0:64, :], scalar1=1.0 / 64, scalar2=None, op0=ALU.mult), ["ps7"], ["mean"])
                dve(lambda: V.tensor_tensor(out=msq2[:], in0=mean[:], in1=mean[:], op=ALU.mult), ["mean"], ["msq2"])
                dve(lambda: V.scalar_tensor_tensor(out=rs[:], in0=ps[0][0:64, :], scalar=1.0 / 64, in1=msq2[:], op0=ALU.mult, op1=ALU.subtract),
                    ["ps0", "msq2"], ["rs"])
                dve(lambda: V.tensor_scalar(out=rs[:], in0=rs[:], scalar1=64e-5, scalar2=None, op0=ALU.add), ["rs"], ["rs"])
                act(lambda: nc.scalar.activation(out=rs[:], in_=rs[:], func=AF.Sqrt), ["rs"], ["rs"])
                dve(lambda: V.reciprocal(out=rs[:], in_=rs[:]), ["rs"], ["rs"])
                dve(lambda: V.tensor_tensor(out=y[:], in0=y[:], in1=mean[:], op=ALU.subtract), ["y", "mean"], ["y"])
                dve(lambda: V.tensor_tensor(out=y[:], in0=y[:], in1=rs[:], op=ALU.mult), ["y", "rs"], ["y"])
                dve(lambda: V.tensor_scalar(out=y[:], in0=y[:], scalar1=vec(4), scalar2=vec(5), op0=ALU.mult, op1=ALU.add), ["y", "pv"], ["y"])
                dve(lambda: V.tensor_tensor(out=y[:], in0=y[:], in1=W["bon"][:], op=ALU.add), ["y", "bon"], ["y"])
                i = (ti * 8 + h) % 2
                dve(lambda: V.tensor_tensor(out=yb[i][:], in0=y[:], in1=g[:], op=ALU.mult), ["y", "g"], ["w_yb%d" % i])
                sc.dma("pool", self.ycatT[1536 + h * 64:1536 + (h + 1) * 64, t0:t0 + T], yb[i][:], reads=["w_yb%d" % i],
                       writes=[("ycatT", ti)])


C_ID = 0
C_ONE = 128
CST_W = 129
CM_MASK = 0
CM_RM = CM_MASK + 4 * T
CM_DIN = CM_RM + 128
CM_QDEC = CM_DIN + 512
CM_KDEC = CM_QDEC + 4 * T
CM_MSK2 = CM_KDEC + 4
CM_MSKT = CM_MSK2 + 8 * 128
CM_I8 = CM_MSKT + 8 * 64
CM_SCAN = CM_I8 + 8 * 64
CM_W = CM_SCAN + T
RET_GAMMA = [float(1.0 - 2.0 ** (-5.0 - h)) for h in range(4)]


def make_consts(S):
    c = np.zeros((128, CST_W), np.float32)
    c[:, C_ID:C_ID + 128] = np.eye(128, dtype=np.float32)
    c[:, C_ONE] = 1.0
    cm = np.zeros((128, CM_W), np.float32)
    p = np.arange(128)[:, None]
    col = np.arange(T)[None, :]
    for j in range(4):
        cm[:, CM_MASK + j * T:CM_MASK + (j + 1) * T] = np.where(col >= j * 128 + p, 0.0, -30000.0)
    rm = np.zeros((128, 128), np.float32)
    for m in range(64):
        rm[m + 64, m] = -1.0
        rm[m, m + 64] = 1.0
    cm[:, CM_RM:CM_RM + 128] = rm
    scale = 128.0 ** -0.5
    i = np.arange(128)
    for h in range(4):
        lgm = np.log(np.float64(RET_GAMMA[h]))
        diff = i[None, :] - i[:, None]
        cm[:, CM_DIN + h * 128:CM_DIN + (h + 1) * 128] = np.where(diff >= 0, scale * np.exp(np.maximum(diff, 0) * lgm), 0.0)
        cm[:, CM_QDEC + h * T:CM_QDEC + (h + 1) * T] = np.tile(scale * np.exp((i + 1.0) * lgm), T // 128)[None, :]
        cm[:, CM_KDEC + h] = np.exp((127.0 - i) * lgm)
    ii = np.arange(64)[:, None]
    tt = np.arange(64)[None, :]
    su = (ii < tt).astype(np.float32)
    ui = (ii <= tt).astype(np.float32)
    sl = (ii > tt).astype(np.float32)
    for c8 in range(8):
        cm[0:64, CM_MSK2 + c8 * 128:CM_MSK2 + c8 * 128 + 64] = su
        cm[0:64, CM_MSK2 + c8 * 128 + 64:CM_MSK2 + (c8 + 1) * 128] = ui
        cm[0:64, CM_MSKT + c8 * 64:CM_MSKT + (c8 + 1) * 64] = sl
        cm[0:64, CM_I8 + c8 * 64:CM_I8 + (c8 + 1) * 64] = np.eye(64, dtype=np.float32)
    cm[:, CM_SCAN:CM_SCAN + T] = (np.arange(T) % 64 != 0).astype(np.float32)[None, :]
    half = 64
    inv = (10000.0 ** (-np.arange(half, dtype=np.float32) / half)).astype(np.float32)
    ang = (np.arange(S, dtype=np.float32)[None, :] * inv[:, None]).astype(np.float32)
    rot = np.empty((2, 128, S), np.float32)
    rot[0, :64] = np.cos(ang); rot[0, 64:] = np.cos(ang)
    rot[1, :64] = np.sin(ang); rot[1, 64:] = np.sin(ang)
    return c, cm, rot


_CACHE = {}


def get_program(S, nq, **kw):
    key = (S, nq, tuple(sorted(kw.items())))
    if key not in _CACHE:
        _CACHE[key] = Builder(S, nq, **kw).build()
    return _CACHE[key]


def kernel(**inputs):
    x = np.asarray(inputs["x"], np.float32)
    B, S, _ = x.shape
    nq = 4
    nc = get_program(S, nq)
    cst, cmix, rot = make_consts(S)
    in_maps = []
    for c in range(8):
        b = c // nq
        m = {k: np.ascontiguousarray(np.asarray(v, np.float32)) for k, v in inputs.items() if k != "x"}
        m["x"] = np.ascontiguousarray(x[b])
        m["cst"] = cst
        m["cmix"] = cmix
        m["rot"] = rot
        in_maps.append(m)
    res = run_bass_kernel_spmd(nc, in_maps, core_ids=list(range(8)))
    out = np.empty((B, S, x.shape[2]), np.float32)
    q = S // nq
    for c in range(8):
        b, k = c // nq, c % nq
        out[b, k * q:(k + 1) * q] = res.results[c]["out"]
    return out
```

```python
from contextlib import ExitStack
import numpy as np
import concourse.bass as bass
import concourse.mybir as mybir
from concourse.bass_utils import run_bass_kernel_spmd

F32 = mybir.dt.float32
BF16 = mybir.dt.bfloat16
AF = mybir.ActivationFunctionType
ALU = mybir.AluOpType

DMIX = 2048
KM = 16
T = 512
EPS = 1e-6
FOXW = 1024
RET0 = 4104
RW0 = 6152
N_IN0 = 8136
N_IN1 = 8200

def _fm_chunks(layer):
    ch = []
    for i in range(8):
        ch.append(("fq%d" % i, i * 128, 128))
    for i in range(8):
        ch.append(("fk%d" % i, 1024 + i * 128, 128))
    for i in range(8):
        ch.append(("fg%d" % i, 3072 + i * 128, 128))
    ch.append(("ff", 4096, 8))
    for i in range(4):
        ch.append(("rq%d" % i, RET0 + i * 128, 128))
    for i in range(4):
        ch.append(("rk%d" % i, RET0 + 512 + i * 128, 128))
    for i in range(4):
        ch.append(("rg%d" % i, RET0 + 1536 + i * 128, 128))
    for i in range(4):
        ch.append(("wr%d" % i, RW0 + i * 128, 128))
    for i in range(4):
        ch.append(("wk%d" % i, RW0 + 512 + i * 128, 128))
    for i in range(4):
        ch.append(("wv%d" % i, RW0 + 1024 + i * 128, 128))
    ch.append(("wwl", RW0 + 1536, 96))
    ch.append(("wal", RW0 + 1632, 96))
    ch.append(("wgl0", RW0 + 1728, 128))
    ch.append(("wgl1", RW0 + 1856, 128))
    if layer > 0:
        ch.append(("wvr", N_IN0, 64))
    return ch

NCH = 54
TM_TILES = [2048, 2560, RET0 + 1024]


class ChunkedRows:
    def __init__(self, aps):
        self.aps = aps

    def __getitem__(self, key):
        rows, cols = key
        c, r0 = rows.start // 128, rows.start % 128
        return self.aps[c][r0:r0 + (rows.stop - rows.start), cols]


class Sched:
    def __init__(self, nc, es):
        self.nc = nc
        self.eng = {"pe": nc.tensor, "act": nc.scalar, "dve": nc.vector, "pool": nc.gpsimd, "sp": nc.sync}
        self.sem = {}
        self.cnt = {}
        self.seen = {e: {} for e in self.eng}
        self.buf = {}
        for e in ("pe", "act", "dve", "pool"):
            self.sem[e] = es.enter_context(nc.semaphore("sem_" + e))
            self.cnt[e] = 0
        self.lanes = {"sp": [], "pool": [], "act": []}
        self.lane_rr = {"sp": 0, "pool": 0, "act": 0}
        for q, n in (("sp", 6), ("pool", 4), ("act", 2)):
            for i in range(n):
                name = "dq_%s%d" % (q, i)
                self.sem[name] = es.enter_context(nc.semaphore(name))
                self.cnt[name] = 0
                self.lanes[q].append(name)

    def _wait(self, e, src, val):
        if val <= 0:
            return
        if self.seen[e].get(src, 0) >= val:
            return
        self.eng[e].wait_ge(self.sem[src], val)
        self.seen[e][src] = val

    def _deps(self, e, reads, writes):
        deps = {}
        for k in reads:
            b = self.buf.get(k)
            if b is not None and b[0] is not None:
                s, v = b[0]
                deps[s] = max(deps.get(s, 0), v)
            if b is not None and isinstance(k, str) and k[:2] == "ps" and k[2:].isdigit():
                for s, v in b[1].items():
                    if s != e:
                        deps[s] = max(deps.get(s, 0), v)
        for k in writes:
            b = self.buf.get(k)
            if b is not None:
                if b[0] is not None:
                    s, v = b[0]
                    deps[s] = max(deps.get(s, 0), v)
                for s, v in b[1].items():
                    deps[s] = max(deps.get(s, 0), v)
        for s, v in deps.items():
            if s == "pe" and e == "pe":
                continue
            self._wait(e, s, v)

    def _mark(self, src, val, reads, writes):
        for k in reads:
            b = self.buf.setdefault(k, [None, {}])
            b[1][src] = val
        for k in writes:
            self.buf[k] = [(src, val), {}]

    def op(self, e, emit, reads=(), writes=()):
        self._deps(e, reads, writes)
        ins = emit()
        ins.then_inc(self.sem[e], 1)
        self.cnt[e] += 1
        self._mark(e, self.cnt[e], reads, writes)
        return ins

    def dma(self, q, out, in_, reads=(), writes=()):
        lanes = self.lanes[q]
        lane = lanes[self.lane_rr[q] % len(lanes)]
        self.lane_rr[q] += 1
        self._wait(q, lane, self.cnt[lane])
        self._deps(q, reads, writes)
        ins = self.eng[q].dma_start(out=out, in_=in_)
        ins.then_inc(self.sem[lane], 16)
        self.cnt[lane] += 16
        self._mark(lane, self.cnt[lane], reads, writes)
        return ins

    def drain(self, e):
        for s, v in self.cnt.items():
            self._wait(e, s, v)


class Builder:
    def __init__(self, S, nq, n_layers=2, debug=False, stop_after=None, D=2048, DFF=5632, mixers=("fox", "ret", "rwkv")):
        self.mixers = mixers
        self.S = S
        self.D, self.DFF, self.KC, self.JC = D, DFF, D // 128, DFF // 128
        self.nq = nq
        self.L = n_layers
        self.NT = S // T
        self.debug = debug
        self.stop_after = stop_after
        self.nc = bass.Bass("TRN2", target_bir_lowering=False)
        self.es = ExitStack()
        self.rr = 0

    def dram_in(self, name, shape):
        return self.nc.dram_tensor(name, list(shape), F32, kind="ExternalInput").ap()

    def dram_tmp(self, name, shape, dt, out=False):
        kind = "ExternalOutput" if (out and self.debug) else "Internal"
        return self.nc.dram_tensor(name, list(shape), dt, kind=kind).ap()

    def sb(self, st, name, shape, dt):
        self.uid = getattr(self, "uid", 0) + 1
        return st.enter_context(self.nc.sbuf_tensor("%s_u%d" % (name, self.uid), list(shape), dt))

    def alt(self):
        self.rr += 1
        return "dve" if self.rr % 2 else "act"

    def copy(self, e, out, in_, reads, writes):
        nc = self.nc
        if e == "act":
            return self.sc.op("act", lambda: nc.scalar.copy(out=out, in_=in_), reads, writes)
        if e == "pool":
            return self.sc.op("pool", lambda: nc.gpsimd.tensor_copy(out=out, in_=in_), reads, writes)
        return self.sc.op("dve", lambda: nc.vector.tensor_copy(out=out, in_=in_), reads, writes)

    def build(self):
        D, KC, DFF, JC = self.D, self.KC, self.DFF, self.JC
        nc, es, S = self.nc, self.es, self.S
        L = self.L
        I = {}
        I["x"] = self.dram_in("x", [S, D])
        I["norm_gains"] = self.dram_in("norm_gains", [2, 6, D])
        I["ffn_w_gu"] = self.dram_in("ffn_w_gu", [2, 2, D, 2 * DFF])
        I["ffn_w_down"] = self.dram_in("ffn_w_down", [2, 2, DFF, D])
        I["w_in_first"] = self.dram_in("w_in_first", [D, N_IN0])
        I["w_in_rest"] = self.dram_in("w_in_rest", [1, D, N_IN1])
        I["w_out"] = self.dram_in("w_out", [2, DMIX, D])
        I["fox_qk_gain"] = self.dram_in("fox_qk_gain", [2, 2, 128])
        I["fox_f_bias"] = self.dram_in("fox_f_bias", [2, 8])
        I["rwkv_mu"] = self.dram_in("rwkv_mu", [2, 1984])
        I["rwkv_vec"] = self.dram_in("rwkv_vec", [2, 6, 512])
        I["rwkv_w2"] = self.dram_in("rwkv_w2", [2, 96, 512])
        I["rwkv_a2"] = self.dram_in("rwkv_a2", [2, 96, 512])
        I["rwkv_g2"] = self.dram_in("rwkv_g2", [2, 256, 512])
        I["rwkv_r_k"] = self.dram_in("rwkv_r_k", [2, 8, 64])
        I["rwkv_v0"] = self.dram_in("rwkv_v0", [1, 512])
        I["rwkv_v2"] = self.dram_in("rwkv_v2", [1, 64, 512])
        I["cst"] = self.dram_in("cst", [128, CST_W])
        I["cmix"] = self.dram_in("cmix", [128, CM_W])
        I["rot"] = self.dram_in("rot", [2, 128, S])
        self.I = I
        rows_out = S // self.nq
        self.out = nc.dram_tensor("out", [rows_out, D], F32, kind="ExternalOutput").ap()
        self.wgu = [[self.dram_tmp("wgu_%d_%d" % (l, f), [JC, 128, KC, 256], BF16) for f in range(2)] for l in range(L)]
        self.wdn = [[self.dram_tmp("wdn_%d_%d" % (l, f), [KC, 128, JC, 128], BF16) for f in range(2)] for l in range(L)]
        self.win = [self.dram_tmp("win_%d" % l, [NCH, 128, KC, 128], BF16) for l in range(L)]
        self.wvm = [self.dram_tmp("wvm_%d" % l, [3, 128, KC, 512], BF16) for l in range(L)]
        self.wo = [self.dram_tmp("wo_%d" % l, [KC, 128, KM, 128], BF16) for l in range(L)]
        self.h1T = self.dram_tmp("h1T", [D, S], F32, out=True)
        self.pT = ChunkedRows([self.dram_tmp("pT%d" % i, [128, S], F32, out=True) for i in range(NCH)])
        self.vtok = self.dram_tmp("vtok", [S, 1536], BF16, out=True)
        self.ycatT = self.dram_tmp("ycatT", [DMIX, S], BF16, out=True)
        self.vfirstT = self.dram_tmp("vfirstT", [512, S], F32)
        self.cumT = self.dram_tmp("cumT", [8, S], F32)

        self.sc = Sched(nc, es)
        sc = self.sc
        self.ps = [es.enter_context(nc.psum_tensor("ps%d" % i, [128, 512], F32)) for i in range(8)]
        self.cst = self.sb(es, "cst_sb", [128, CST_W], F32)
        sc.dma("sp", self.cst[:], I["cst"][:, :], writes=["cst"])
        self.ident = self.cst[:, C_ID:C_ID + 128]
        self.one_col = self.cst[:, C_ONE:C_ONE + 1]
        self.ones_bf = self.sb(es, "ones_bf", [128, 128], BF16)
        sc.op("pool", lambda: nc.gpsimd.memset(self.ones_bf[:], 1.0), writes=["ones_bf"])
        self.gcol = self.sb(es, "gcol", [128, 12, KC], F32)
        self.ghalf = self.sb(es, "ghalf", [128, 12, KC], F32)
        with nc.allow_non_contiguous_dma(reason="tiny gain vectors, column layout"):
            for gl in range(2):
                for gi in range(6):
                    sc.dma("pool", self.gcol[:, gl * 6 + gi, :], I["norm_gains"][gl, gi].rearrange("(c p) -> p c", p=128))
            for lane in sc.lanes["pool"]:
                sc._wait("dve", lane, sc.cnt[lane])
        sc.op("dve", lambda: nc.vector.tensor_scalar(out=self.ghalf[:], in0=self.gcol[:], scalar1=0.5, scalar2=None,
                                                      op0=ALU.mult), reads=["gcol"], writes=["ghalf"])
        self.prep_weights()
        if self.stop_after == "prep":
            return self.finish()
        for l in range(L):
            if l == 0:
                self.token_phase(first=True, layer=0)
            if self.stop_after == "A%d" % l:
                return self.finish()
            self.mixer_phase(l)
            if self.stop_after == "B%d" % l:
                return self.finish()
            self.token_phase(first=False, layer=l)
        return self.finish()

    def finish(self):
        for e in ("sp", "pool", "act", "dve", "pe"):
            self.sc.drain(e)
        self.es.close()
        return self.nc

    def prep_one(self, st, W, Kdim, col0, ncols, dst, tw, dcol0=0):
        nc, sc = self.nc, self.sc
        kcs = Kdim // 128
        CB = 2048
        for kc in range(kcs):
            c = 0
            while c < ncols:
                cb = min(CB, ncols - c)
                i = self.pp % 2
                self.pp += 1
                sf, sbf = self.pstg[i], self.pstb[i]
                sc.dma("sp", sf[:, 0:cb], W[kc * 128:(kc + 1) * 128, col0 + c:col0 + c + cb], writes=["pstg%d" % i])
                eng = ("dve", "act", "pool")[self.pp % 3]
                self.copy(eng, sbf[:, 0:cb], sf[:, 0:cb], ["pstg%d" % i], ["pstb%d" % i])
                t0 = c // tw
                nfull = cb // tw
                if nfull > 0:
                    sc.dma("pool", dst[t0:t0 + nfull, :, kc, dcol0:dcol0 + tw].rearrange("t p c -> p t c"),
                           sbf[:, 0:nfull * tw].rearrange("p (t c) -> p t c", c=tw), reads=["pstb%d" % i])
                rem = cb - nfull * tw
                if rem > 0:
                    sc.dma("pool", dst[t0 + nfull, :, kc, dcol0:dcol0 + rem], sbf[:, nfull * tw:cb],
                           reads=["pstb%d" % i])
                c += cb

    def prep_weights(self):
        D, KC, DFF, JC = self.D, self.KC, self.DFF, self.JC
        nc, sc, I = self.nc, self.sc, self.I
        self.pp = 0
        with ExitStack() as st:
            self.pstg = [self.sb(st, "pstg%d" % i, [128, 2048], F32) for i in range(2)]
            self.pstb = [self.sb(st, "pstb%d" % i, [128, 2048], BF16) for i in range(2)]
            with nc.allow_non_contiguous_dma(reason="weight re-tiling, 256B+ runs"):
                for l in range(self.L):
                    for f in range(2):
                        Wgu = I["ffn_w_gu"][l, f]
                        self.prep_one(st, Wgu, D, 0, DFF, self.wgu[l][f], 128, 0)
                        self.prep_one(st, Wgu, D, DFF, DFF, self.wgu[l][f], 128, 128)
                        self.prep_one(st, I["ffn_w_down"][l, f], DFF, 0, D, self.wdn[l][f], 128, 0)
                    Win = I["w_in_first"] if l == 0 else I["w_in_rest"][l - 1]
                    for ci, (nm, c0, w) in enumerate(_fm_chunks(l)):
                        self.prep_one(st, Win, D, c0, w, self.win[l][ci:ci + 1], 128, 0)
                    for ti, c0 in enumerate(TM_TILES):
                        self.prep_one(st, Win, D, c0, 512, self.wvm[l][ti:ti + 1], 512, 0)
                    self.prep_one(st, I["w_out"][l], DMIX, 0, D, self.wo[l], 128, 0)
            self.barrier()

    def barrier(self):
        for e in ("sp", "pool", "act", "dve", "pe"):
            self.sc.drain(e)

    def load_w(self, src, nelem, shape3):
        i = self.wrr % len(self.wsl)
        self.wrr += 1
        key = "wsl%d" % i
        a, b = shape3
        ap = self.wsl[i][:, 0:a * b].rearrange("p (a b) -> p a b", b=b)
        self.sc.dma("sp", ap, src, writes=[key])
        return ap, key

    def stats_finish(self, ps_key, ps_ap, dim):
        nc, sc = self.nc, self.sc
        sc.op("dve", lambda: nc.vector.tensor_scalar(out=self.ms[:], in0=ps_ap, scalar1=1.0 / dim, scalar2=EPS,
                                                      op0=ALU.mult, op1=ALU.add), reads=[ps_key], writes=["ms"])
        sc.op("act", lambda: nc.scalar.activation(out=self.ms[:], in_=self.ms[:], func=AF.Sqrt), reads=["ms"], writes=["ms"])
        sc.op("dve", lambda: nc.vector.reciprocal(out=self.rstd[:], in_=self.ms[:]), reads=["ms"], writes=["rstd"])

    def norm_to_uT(self, gi):
        D, KC, DFF, JC = self.D, self.KC, self.DFF, self.JC
        nc, sc = self.nc, self.sc
        for kc in range(KC):
            i = kc % 2
            sc.op("act", lambda: nc.scalar.activation(out=self.sq[i][:], in_=self.hT[:, kc, :], func=AF.Square),
                  reads=["hT%d" % kc], writes=["sq%d" % i])
            sc.op("pe", lambda: nc.tensor.matmul(self.ps[6][:], lhsT=self.ones_bf[:], rhs=self.sq[i][:],
                                                  start=(kc == 0), stop=(kc == KC - 1)),
                  reads=["sq%d" % i, "ones_bf"], writes=["ps6"])
        self.stats_finish("ps6", self.ps[6][:], D)
        for kc in range(KC):
            sc.op("dve", lambda: nc.vector.scalar_tensor_tensor(out=self.uT[:, kc, :], in0=self.hT[:, kc, :],
                                                                 scalar=self.gcol[:, gi, kc:kc + 1], in1=self.rstd[:],
                                                                 op0=ALU.mult, op1=ALU.mult),
                  reads=["hT%d" % kc, "rstd", "gcol"], writes=["uT"])

    def resid_update(self, gi, half):
        D, KC, DFF, JC = self.D, self.KC, self.DFF, self.JC
        nc, sc = self.nc, self.sc
        self.stats_finish("ps6", self.ps[6][:], D)
        g = self.ghalf if half else self.gcol
        for m in range(KC):
            sc.op("dve", lambda: nc.vector.scalar_tensor_tensor(out=self.fT[:, m, :], in0=self.fT[:, m, :],
                                                                 scalar=g[:, gi, m:m + 1], in1=self.rstd[:],
                                                                 op0=ALU.mult, op1=ALU.mult),
                  reads=["fT%d" % m, "rstd", "gcol", "ghalf"], writes=["fT%d" % m])
            sc.op("pool", lambda: nc.gpsimd.tensor_tensor(out=self.hT[:, m, :], in0=self.hT[:, m, :], in1=self.fT[:, m, :],
                                                          op=ALU.add),
                  reads=["fT%d" % m, "hT%d" % m], writes=["hT%d" % m])

    def proj_to_fT(self, wt, kcs, inT, in_key):
        D, KC, DFF, JC = self.D, self.KC, self.DFF, self.JC
        nc, sc = self.nc, self.sc
        for m in range(KC):
            w, wk = self.load_w(wt[m], kcs * 128, (kcs, 128))
            pb = 4 + (m % 2)
            for k in range(kcs):
                sc.op("pe", lambda: nc.tensor.matmul(self.ps[pb][:], lhsT=w[:, k, :], rhs=inT[:, k, :],
                                                      start=(k == 0), stop=(k == kcs - 1)),
                      reads=[wk, in_key], writes=["ps%d" % pb])
            sc.op("dve", lambda: nc.vector.tensor_copy(out=self.fT[:, m, :], in_=self.ps[pb][:]),
                  reads=["ps%d" % pb], writes=["fT%d" % m])
            i = m % 2
            sc.op("act", lambda: nc.scalar.activation(out=self.sq[i][:], in_=self.fT[:, m, :], func=AF.Square),
                  reads=["fT%d" % m], writes=["sq%d" % i])
            sc.op("pe", lambda: nc.tensor.matmul(self.ps[6][:], lhsT=self.ones_bf[:], rhs=self.sq[i][:],
                                                  start=(m == 0), stop=(m == KC - 1)),
                  reads=["sq%d" % i, "ones_bf"], writes=["ps6"])

    def ffn(self, l, f, g_in, g_out):
        D, KC, DFF, JC = self.D, self.KC, self.DFF, self.JC
        nc, sc = self.nc, self.sc
        self.norm_to_uT(g_in)
        wgu = self.wgu[l][f]
        for j in range(JC):
            w, wk = self.load_w(wgu[j], KC * 256, (KC, 256))
            pg, pu = (j % 2), 2 + (j % 2)
            for k in range(KC):
                sc.op("pe", lambda: nc.tensor.matmul(self.ps[pg][:], lhsT=w[:, k, 0:128], rhs=self.uT[:, k, :],
                                                      start=(k == 0), stop=(k == KC - 1)),
                      reads=[wk, "uT"], writes=["ps%d" % pg])
            for k in range(KC):
                sc.op("pe", lambda: nc.tensor.matmul(self.ps[pu][:], lhsT=w[:, k, 128:256], rhs=self.uT[:, k, :],
                                                      start=(k == 0), stop=(k == KC - 1)),
                      reads=[wk, "uT"], writes=["ps%d" % pu])
            i = j % 2
            sc.op("act", lambda: nc.scalar.activation(out=self.sg[i][:], in_=self.ps[pg][:], func=AF.Silu),
                  reads=["ps%d" % pg], writes=["sg%d" % i])
            sc.op("dve", lambda: nc.vector.tensor_tensor(out=self.actT[:, j, :], in0=self.sg[i][:], in1=self.ps[pu][:],
                                                          op=ALU.mult),
                  reads=["sg%d" % i, "ps%d" % pu], writes=["actT"])
        import os
        bis = int(os.environ.get("BISECT", "99"))
        if bis == 4:
            return
        self.proj_to_fT(self.wdn[l][f], JC, self.actT, "actT")
        if bis == 5:
            return
        self.resid_update(g_out, True)

    def in_proj(self, l, t0):
        D, KC, DFF, JC = self.D, self.KC, self.DFF, self.JC
        nc, sc = self.nc, self.sc
        chunks = _fm_chunks(l)
        for ci, (nm, c0, wd) in enumerate(chunks):
            w, wk = self.load_w(self.win[l][ci][:, :, 0:wd], KC * wd, (KC, wd))
            pb = ci % 4
            for k in range(KC):
                sc.op("pe", lambda: nc.tensor.matmul(self.ps[pb][0:wd, :], lhsT=w[:, k, 0:wd], rhs=self.uT[:, k, :],
                                                      start=(k == 0), stop=(k == KC - 1)),
                      reads=[wk, "uT"], writes=["ps%d" % pb])
            i = ci % 2
            self.copy(self.alt(), self.stg[i][0:wd, :], self.ps[pb][0:wd, :], ["ps%d" % pb], ["stg%d" % i])
            sc.dma("pool", self.pT[ci * 128:ci * 128 + wd, t0:t0 + T], self.stg[i][0:wd, :], reads=["stg%d" % i],
                   writes=[("pT", ci, t0 // T)])
        for ti in range(3):
            w, wk = self.load_w(self.wvm[l][ti], KC * 512, (KC, 512))
            for tb in range(T // 128):
                pb = (ti * 4 + tb) % 4
                for k in range(KC):
                    sc.op("pe", lambda: nc.tensor.matmul(self.ps[pb][:], lhsT=self.uT[:, k, tb * 128:(tb + 1) * 128],
                                                          rhs=w[:, k, :], start=(k == 0), stop=(k == KC - 1)),
                          reads=[wk, "uT"], writes=["ps%d" % pb])
                i = (ti * 4 + tb) % 2
                self.copy(self.alt(), self.stgb[i][:], self.ps[pb][:], ["ps%d" % pb], ["stgb%d" % i])
                sc.dma("pool", self.vtok[t0 + tb * 128:t0 + (tb + 1) * 128, ti * 512:(ti + 1) * 512], self.stgb[i][:],
                       reads=["stgb%d" % i], writes=[("vtok", t0 // T)])

    def load_x_tile(self, t0):
        D, KC, DFF, JC = self.D, self.KC, self.DFF, self.JC
        nc, sc = self.nc, self.sc
        for tb in range(T // 128):
            i = tb % 2
            sc.dma("pool", self.xin[i][:], self.I["x"][t0 + tb * 128:t0 + (tb + 1) * 128, :], writes=["xin%d" % i])
            for fg in range(KC // 4):
                pb = fg % 4
                for q in range(4):
                    fc = fg * 4 + q
                    sc.op("pe", lambda: nc.tensor.transpose(out=self.ps[pb][:, q * 128:(q + 1) * 128],
                                                             in_=self.xin[i][:, fc * 128:(fc + 1) * 128], identity=self.ident),
                          reads=["xin%d" % i, "cst"], writes=["ps%d" % pb])
                self.copy(self.alt(), self.hT[:, fg * 4:fg * 4 + 4, tb * 128:(tb + 1) * 128],
                          self.ps[pb][:].rearrange("p (q t) -> p q t", q=4), ["ps%d" % pb],
                          ["hT%d" % (fg * 4 + q) for q in range(4)])

    def store_out_tile(self, r0):
        D, KC, DFF, JC = self.D, self.KC, self.DFF, self.JC
        nc, sc = self.nc, self.sc
        for tb in range(T // 128):
            i = tb % 2
            for fg in range(KC // 4):
                pb = fg % 4
                for q in range(4):
                    fc = fg * 4 + q
                    sc.op("pe", lambda: nc.tensor.transpose(out=self.ps[pb][:, q * 128:(q + 1) * 128],
                                                             in_=self.hT[:, fc, tb * 128:(tb + 1) * 128], identity=self.ident),
                          reads=["hT%d" % fc, "cst"], writes=["ps%d" % pb])
                self.copy(self.alt(), self.xin[i][:, fg * 512:(fg + 1) * 512], self.ps[pb][:], ["ps%d" % pb], ["xin%d" % i])
            sc.dma("pool", self.out[r0 + tb * 128:r0 + (tb + 1) * 128, :], self.xin[i][:], reads=["xin%d" % i], writes=["out"])

    def token_phase(self, first, layer):
        D, KC, DFF, JC = self.D, self.KC, self.DFF, self.JC
        nc, sc, S = self.nc, self.sc, self.S
        last = (not first) and (layer == self.L - 1)
        with ExitStack() as st:
            self.hT = self.sb(st, "hT", [128, KC, T], F32)
            self.uT = self.sb(st, "uT", [128, max(KC, KM), T], BF16)
            self.actT = self.sb(st, "actT", [128, JC, T], BF16)
            self.fT = self.sb(st, "fT", [128, KC, T], F32)
            self.wsl = [self.sb(st, "wsl%d" % i, [128, 8192], BF16) for i in range(3)]
            self.wrr = 0
            self.stg = [self.sb(st, "stg%d" % i, [128, T], F32) for i in range(2)]
            self.stgb = [self.sb(st, "stgb%d" % i, [128, T], BF16) for i in range(2)]
            self.sq = [self.sb(st, "sq%d" % i, [128, T], BF16) for i in range(2)]
            self.sg = [self.sb(st, "sg%d" % i, [128, T], F32) for i in range(2)]
            self.rstd = self.sb(st, "rstd", [128, T], F32)
            self.ms = self.sb(st, "ms", [128, T], F32)
            self.xin = [self.sb(st, "xin%d" % i, [128, D], F32) for i in range(2)]
            hkeys = ["hT%d" % k for k in range(KC)]
            if last:
                ntiles = (S // self.nq) // T
                if self.nq > 1:
                    pid = nc.gpsimd.partition_id()
                    base = (pid % self.nq) * (S // self.nq)
                else:
                    base = 0
            else:
                ntiles = self.NT
                base = 0
            for ti in range(ntiles):
                t0 = ti * T
                if first:
                    import os
                    bis = int(os.environ.get("BISECT", "99"))
                    self.load_x_tile(t0)
                    if bis == 1:
                        continue
                    if bis == 2:
                        self.norm_to_uT(0)
                        continue
                    self.ffn(0, 0, 0, 1)
                    if bis == 3:
                        continue
                    sc.dma("pool", self.h1T.rearrange("(c p) s -> p c s", p=128)[:, :, t0:t0 + T], self.hT[:], reads=hkeys,
                           writes=[("h1T", ti)])
                    self.norm_to_uT(2)
                    self.in_proj(0, t0)
                    continue
                if last and self.nq > 1:
                    col = bass.ds(base + t0, T)
                else:
                    col = slice(t0, t0 + T)
                hsrc = self.h1T.rearrange("(c p) s -> p c s", p=128)[:, :, col]
                ysrc = self.ycatT.rearrange("(c p) s -> p c s", p=128)[:, :, col]
                rkeys = [("h1T", i) for i in range(self.NT)] if last else [("h1T", ti)]
                ykeys = [("ycatT", i) for i in range(self.NT)] if last else [("ycatT", ti)]
                sc.dma("pool", self.hT[:], hsrc, reads=rkeys, writes=hkeys)
                sc.dma("pool", self.uT[:, 0:KM, :], ysrc, reads=ykeys, writes=["uT"])
                self.proj_to_fT(self.wo[layer], KM, self.uT, "uT")
                self.resid_update(layer * 6 + 3, False)
                self.ffn(layer, 1, layer * 6 + 4, layer * 6 + 5)
                if last:
                    self.store_out_tile(t0)
                else:
                    l2 = layer + 1
                    self.ffn(l2, 0, l2 * 6 + 0, l2 * 6 + 1)
                    sc.dma("pool", self.h1T.rearrange("(c p) s -> p c s", p=128)[:, :, t0:t0 + T], self.hT[:], reads=hkeys,
                           writes=[("h1T", ti)])
                    self.norm_to_uT(l2 * 6 + 2)
                    self.in_proj(l2, t0)
            for e in ("sp", "pool", "act", "dve", "pe"):
                sc.drain(e)

    def mixer_phase(self, l):
        which = self.mixers
        if "fox" in which:
            with ExitStack() as st:
                self.fox_phase(l, st)
                self.barrier()
        if "ret" in which:
            with ExitStack() as st:
                self.ret_phase(l, st)
                self.barrier()
        if "rwkv" in which:
            with ExitStack() as st:
                self.rwkv_phase(l, st)
                self.barrier()

    def small_stats(self, src, sqk, dim, eps, psb=7):
        nc, sc = self.nc, self.sc
        sc.op("act", lambda: nc.scalar.activation(out=self.msq[:], in_=src, func=AF.Square), reads=[sqk], writes=["msq"])
        sc.op("pe", lambda: nc.tensor.matmul(self.ps[psb][:], lhsT=self.ones_bf[:], rhs=self.msq[:], start=True, stop=True),
              reads=["msq", "ones_bf"], writes=["ps%d" % psb])
        sc.op("dve", lambda: nc.vector.tensor_scalar(out=self.mms[:], in0=self.ps[psb][:], scalar1=1.0 / dim, scalar2=eps,
                                                      op0=ALU.mult, op1=ALU.add), reads=["ps%d" % psb], writes=["mms"])
        sc.op("act", lambda: nc.scalar.activation(out=self.mms[:], in_=self.mms[:], func=AF.Sqrt), reads=["mms"], writes=["mms"])
        sc.op("dve", lambda: nc.vector.reciprocal(out=self.mrs[:], in_=self.mms[:]), reads=["mms"], writes=["mrs"])

    def fox_phase(self, l, st):
        nc, sc, S, I = self.nc, self.sc, self.S, self.I
        NB, NQT = S // 128, S // T
        SEG = min(2048, S)
        pT, vtok = self.pT, self.vtok
        fl = [self.sb(st, "fl%d" % i, [8, SEG], F32) for i in range(2)]
        cs = [self.sb(st, "cs%d" % i, [8, SEG], F32) for i in range(2)]
        one8 = self.sb(st, "one8", [8, SEG], F32)
        ccol = self.sb(st, "ccol", [128, NB, 8], F32)
        nfb = self.sb(st, "nfb", [8, 1], F32)
        gq = self.sb(st, "gq", [128, 2], F32)
        qn = self.sb(st, "qn", [128, S], BF16)
        kn = self.sb(st, "kn", [128, S], BF16)
        vv = self.sb(st, "vv", [128, NB, 128], BF16)
        masks = self.sb(st, "masks", [128, 4, T], F32)
        bq = [self.sb(st, "bq%d" % i, [128, T], F32) for i in range(2)]
        lg = [self.sb(st, "lg%d" % i, [128, T], F32) for i in range(2)]
        pp = [self.sb(st, "pp%d" % i, [128, T], BF16) for i in range(2)]
        x32 = [self.sb(st, "x32_%d" % i, [128, T], F32) for i in range(2)]
        self.msq = self.sb(st, "msq", [128, T], BF16)
        self.mms = self.sb(st, "mms", [128, T], F32)
        self.mrs = self.sb(st, "mrs", [128, T], F32)
        rd = self.sb(st, "rd", [128, T], F32)
        o32 = self.sb(st, "o32", [128, T], F32)
        yb = [self.sb(st, "yb%d" % i, [128, T], BF16) for i in range(2)]
        ps = self.ps
        sc.dma("pool", masks[:], I["cmix"][:, CM_MASK:CM_MASK + 4 * T].rearrange("p (j t) -> p j t", j=4), writes=["masks"])
        sc.op("pool", lambda: nc.gpsimd.memset(one8[:], 1.0), writes=["one8"])
        with nc.allow_non_contiguous_dma(reason="tiny per-head vectors"):
            sc.dma("pool", nfb[:], I["fox_f_bias"][l].rearrange("(p o) -> p o", o=1), writes=["nfb"])
            sc.dma("pool", gq[:], I["fox_qk_gain"][l].rearrange("j p -> p j"), writes=["gq"])
        sc.op("dve", lambda: nc.vector.tensor_scalar(out=nfb[:], in0=nfb[:], scalar1=-1.0, scalar2=None, op0=ALU.mult),
              reads=["nfb"], writes=["nfb"])
        sc.op("dve", lambda: nc.vector.tensor_scalar(out=gq[:, 0:1], in0=gq[:, 0:1], scalar1=float(128.0 ** -0.5), scalar2=None,
                                                      op0=ALU.mult), reads=["gq"], writes=["gq"])
        for sg in range(S // SEG):
            i = sg % 2
            c0 = sg * SEG
            sc.dma("pool", fl[i][:], pT[24 * 128:24 * 128 + 8, c0:c0 + SEG], reads=[("pT", 24, t) for t in range(NQT)],
                   writes=["fl%d" % i])
            sc.op("act", lambda: nc.scalar.activation(out=fl[i][:], in_=fl[i][:], func=AF.Exp, bias=nfb[:, 0:1], scale=-1.0),
                  reads=["fl%d" % i, "nfb"], writes=["fl%d" % i])
            sc.op("act", lambda: nc.scalar.activation(out=fl[i][:], in_=fl[i][:], func=AF.Ln, bias=self.one_col[0:8, 0:1]),
                  reads=["fl%d" % i], writes=["fl%d" % i])
            init = 0.0 if sg == 0 else cs[1 - i][:, SEG - 1:SEG]
            sc.op("dve", lambda: nc.vector.tensor_tensor_scan(out=cs[i][:], data0=one8[:], data1=fl[i][:], initial=init,
                                                               op0=ALU.mult, op1=ALU.add),
                  reads=["fl%d" % i, "one8", "cs%d" % (1 - i)], writes=["cs%d" % i])
            sc.op("dve", lambda: nc.vector.tensor_scalar(out=fl[i][:], in0=cs[i][:], scalar1=-1.0, scalar2=None, op0=ALU.mult),
                  reads=["cs%d" % i], writes=["fl%d" % i])
            sc.dma("pool", self.cumT[:, c0:c0 + SEG], fl[i][:], reads=["fl%d" % i], writes=["cumT"])
            nb = SEG // 128
            for b in range(nb):
                sc.op("pe", lambda: nc.tensor.transpose(out=ps[7][:, b * 8:(b + 1) * 8], in_=cs[i][0:8, b * 128:(b + 1) * 128],
                                                         identity=self.ident[0:8, 0:8]),
                      reads=["cs%d" % i, "cst"], writes=["ps7"])
            sc.op("dve", lambda: nc.vector.tensor_copy(out=ccol[:, sg * nb:(sg + 1) * nb, :],
                                                        in_=ps[7][:, 0:nb * 8].rearrange("p (b e) -> p b e", e=8)),
                  reads=["ps7"], writes=["ccol"])
        for h in range(8):
            for ti in range(NQT):
                t0 = ti * T
                for (chunk, dst, dk, gi) in ((h, qn, "qn", 0), (8 + h, kn, "kn", 1)):
                    i = (2 * ti + gi) % 2
                    sc.dma("pool", x32[i][:], pT[chunk * 128:(chunk + 1) * 128, t0:t0 + T], reads=[("pT", chunk, ti)],
                           writes=["x32_%d" % i])
                    self.small_stats(x32[i][:], "x32_%d" % i, 128.0, EPS)
                    sc.op("dve", lambda: nc.vector.scalar_tensor_tensor(out=dst[:, t0:t0 + T], in0=x32[i][:], scalar=gq[:, gi:gi + 1],
                                                                         in1=self.mrs[:], op0=ALU.mult, op1=ALU.mult),
                          reads=["x32_%d" % i, "gq", "mrs"], writes=[dk])
            nsp = max(1, NB // 32)
            bp = NB // nsp
            for j in range(nsp):
                sc.dma("pool", vv[:, j * bp:(j + 1) * bp, :],
                       vtok[j * bp * 128:(j + 1) * bp * 128, h * 128:(h + 1) * 128].rearrange("(b p) d -> p b d", p=128),
                       reads=[("vtok", t) for t in range(NQT)], writes=["vv"])
            for ti in range(NQT):
                t0 = ti * T
                i = ti % 2
                sc.dma("pool", bq[i][:], self.cumT[h:h + 1, t0:t0 + T].partition_broadcast(128), reads=["cumT"], writes=["bq%d" % i])
                nkb = 4 * (ti + 1)
                po, pd = 2 + i, 4 + i
                for kb in range(nkb):
                    j = kb % 2
                    sc.op("pe", lambda: nc.tensor.matmul(ps[j][:], lhsT=kn[:, kb * 128:(kb + 1) * 128], rhs=qn[:, t0:t0 + T],
                                                          start=True, stop=True), reads=["kn", "qn"], writes=["ps%d" % j])
                    sc.op("dve", lambda: nc.vector.tensor_tensor(out=lg[j][:], in0=ps[j][:], in1=bq[i][:], op=ALU.add),
                          reads=["ps%d" % j, "bq%d" % i], writes=["lg%d" % j])
                    if kb >= 4 * ti:
                        sc.op("pool", lambda: nc.gpsimd.tensor_tensor(out=lg[j][:], in0=lg[j][:], in1=masks[:, kb - 4 * ti, :],
                                                                      op=ALU.add), reads=["lg%d" % j, "masks"], writes=["lg%d" % j])
                    sc.op("act", lambda: nc.scalar.activation(out=pp[j][:], in_=lg[j][:], func=AF.Exp, bias=ccol[:, kb, h:h + 1]),
                          reads=["lg%d" % j, "ccol"], writes=["pp%d" % j])
                    sc.op("pe", lambda: nc.tensor.matmul(ps[po][:], lhsT=vv[:, kb, :], rhs=pp[j][:], start=(kb == 0),
                                                          stop=(kb == nkb - 1)), reads=["vv", "pp%d" % j], writes=["ps%d" % po])
                    sc.op("pe", lambda: nc.tensor.matmul(ps[pd][:], lhsT=self.ones_bf[:], rhs=pp[j][:], start=(kb == 0),
                                                          stop=(kb == nkb - 1)), reads=["ones_bf", "pp%d" % j], writes=["ps%d" % pd])
                sc.op("dve", lambda: nc.vector.reciprocal(out=rd[:], in_=ps[pd][:]), reads=["ps%d" % pd], writes=["rd"])
                sc.op("dve", lambda: nc.vector.tensor_tensor(out=o32[:], in0=ps[po][:], in1=rd[:], op=ALU.mult),
                      reads=["ps%d" % po, "rd"], writes=["o32"])
                gi_ = ti % 2
                sc.dma("pool", x32[gi_][:], pT[(16 + h) * 128:(17 + h) * 128, t0:t0 + T], reads=[("pT", 16 + h, ti)],
                       writes=["x32_%d" % gi_])
                sc.op("act", lambda: nc.scalar.activation(out=x32[gi_][:], in_=x32[gi_][:], func=AF.Sigmoid),
                      reads=["x32_%d" % gi_], writes=["x32_%d" % gi_])
                sc.op("dve", lambda: nc.vector.tensor_tensor(out=yb[i][:], in0=o32[:], in1=x32[gi_][:], op=ALU.mult),
                      reads=["o32", "x32_%d" % gi_], writes=["yb%d" % i])
                sc.dma("pool", self.ycatT[h * 128:(h + 1) * 128, t0:t0 + T], yb[i][:], reads=["yb%d" % i], writes=[("ycatT", ti)])

    def ret_phase(self, l, st):
        nc, sc, S, I = self.nc, self.sc, self.S, self.I
        NQT = S // T
        pT, vtok = self.pT, self.vtok
        ps = self.ps
        rm = self.sb(st, "rm", [128, 128], F32)
        din = self.sb(st, "din", [128, 4, 128], F32)
        qdec = self.sb(st, "qdec", [128, 4, T], F32)
        kdec = self.sb(st, "kdec", [128, 4], F32)
        q32 = self.sb(st, "q32", [128, T], F32)
        k32 = self.sb(st, "k32", [128, T], F32)
        cst_ = self.sb(st, "rcos", [128, T], F32)
        snt = self.sb(st, "rsin", [128, T], F32)
        t1 = self.sb(st, "rt1", [128, T], F32)
        t2 = self.sb(st, "rt2", [128, T], F32)
        qr = self.sb(st, "qr", [128, T], F32)
        kr = self.sb(st, "kr", [128, T], F32)
        qd = self.sb(st, "qd", [128, T], BF16)
        vt = self.sb(st, "rvt", [128, 4, 128], BF16)
        PT = [self.sb(st, "rPT%d" % i, [128, 128], BF16) for i in range(2)]
        kd = [self.sb(st, "rkd%d" % i, [128, 128], BF16) for i in range(2)]
        st32 = [self.sb(st, "rst32_%d" % h, [128, 128], F32) for h in range(4)]
        stbf = [self.sb(st, "rstbf_%d" % h, [128, 128], BF16) for h in range(4)]
        o32 = self.sb(st, "ro32", [128, T], F32)
        g32 = self.sb(st, "rg32", [128, T], F32)
        yb = [self.sb(st, "ryb%d" % i, [128, T], BF16) for i in range(2)]
        self.msq = self.sb(st, "msq_r", [128, T], BF16)
        self.mms = self.sb(st, "mms_r", [128, T], F32)
        self.mrs = self.sb(st, "mrs_r", [128, T], F32)
        cm = I["cmix"]
        sc.dma("pool", rm[:], cm[:, CM_RM:CM_RM + 128], writes=["rm"])
        sc.dma("pool", din[:], cm[:, CM_DIN:CM_DIN + 512].rearrange("p (h i) -> p h i", h=4), writes=["din"])
        sc.dma("pool", qdec[:], cm[:, CM_QDEC:CM_QDEC + 4 * T].rearrange("p (h i) -> p h i", h=4), writes=["qdec"])
        sc.dma("pool", kdec[:], cm[:, CM_KDEC:CM_KDEC + 4], writes=["kdec"])
        for h in range(4):
            sc.op("pool", lambda: nc.gpsimd.memset(st32[h][:], 0.0), writes=["rst32_%d" % h])
            sc.op("pool", lambda: nc.gpsimd.memset(stbf[h][:], 0.0), writes=["rstbf_%d" % h])
        for ti in range(NQT):
            t0 = ti * T
            sc.dma("pool", cst_[:], I["rot"][0, :, t0:t0 + T], writes=["rcos"])
            sc.dma("pool", snt[:], I["rot"][1, :, t0:t0 + T], writes=["rsin"])
            for h in range(4):
                gam = RET_GAMMA[h]
                sc.dma("pool", q32[:], pT[(25 + h) * 128:(26 + h) * 128, t0:t0 + T], reads=[("pT", 25 + h, ti)], writes=["q32"])
                sc.dma("pool", k32[:], pT[(29 + h) * 128:(30 + h) * 128, t0:t0 + T], reads=[("pT", 29 + h, ti)], writes=["k32"])
                sc.dma("pool", vt[:], vtok[t0:t0 + T, 1024 + h * 128:1024 + (h + 1) * 128].rearrange("(c p) e -> p c e", p=128),
                       reads=[("vtok", ti)], writes=["rvt"])
                for (src, sk, dst, dk) in ((q32, "q32", qr, "qr"), (k32, "k32", kr, "kr")):
                    sc.op("pe", lambda: nc.tensor.matmul(ps[0][:], lhsT=rm[:], rhs=src[:], start=True, stop=True),
                          reads=["rm", sk], writes=["ps0"])
                    sc.op("dve", lambda: nc.vector.tensor_tensor(out=t1[:], in0=src[:], in1=cst_[:], op=ALU.mult),
                          reads=[sk, "rcos"], writes=["rt1"])
                    sc.op("dve", lambda: nc.vector.tensor_tensor(out=t2[:], in0=ps[0][:], in1=snt[:], op=ALU.mult),
                          reads=["ps0", "rsin"], writes=["rt2"])
                    sc.op("pool", lambda: nc.gpsimd.tensor_tensor(out=dst[:], in0=t1[:], in1=t2[:], op=ALU.add),
                          reads=["rt1", "rt2"], writes=[dk])
                sc.op("dve", lambda: nc.vector.tensor_tensor(out=qd[:], in0=qr[:], in1=qdec[:, h, :], op=ALU.mult),
                      reads=["qr", "qdec"], writes=["qd"])
                for c in range(T // 128):
                    cs_ = slice(c * 128, (c + 1) * 128)
                    j = c % 2
                    sc.op("pe", lambda: nc.tensor.matmul(ps[1][:, 0:128], lhsT=kr[:, cs_], rhs=qr[:, cs_], start=True, stop=True),
                          reads=["kr", "qr"], writes=["ps1"])
                    sc.op("dve", lambda: nc.vector.tensor_tensor(out=PT[j][:], in0=ps[1][:, 0:128], in1=din[:, h, :], op=ALU.mult),
                          reads=["ps1", "din"], writes=["rPT%d" % j])
                    sc.op("pe", lambda: nc.tensor.matmul(ps[2][:, cs_], lhsT=vt[:, c, :], rhs=PT[j][:], start=True, stop=False),
                          reads=["rvt", "rPT%d" % j], writes=["ps2"])
                    sc.op("pe", lambda: nc.tensor.matmul(ps[2][:, cs_], lhsT=stbf[h][:], rhs=qd[:, cs_], start=False, stop=True),
                          reads=["rstbf_%d" % h, "qd"], writes=["ps2"])
                    sc.op("pe", lambda: nc.tensor.transpose(out=ps[3][:, 0:128], in_=kr[:, cs_], identity=self.ident),
                          reads=["kr", "cst"], writes=["ps3"])
                    sc.op("act", lambda: nc.scalar.activation(out=kd[j][:], in_=ps[3][:, 0:128], func=AF.Copy, scale=kdec[:, h:h + 1]),
                          reads=["ps3", "kdec"], writes=["rkd%d" % j])
                    sc.op("pe", lambda: nc.tensor.matmul(ps[4][:, 0:128], lhsT=kd[j][:], rhs=vt[:, c, :], start=True, stop=True),
                          reads=["rkd%d" % j, "rvt"], writes=["ps4"])
                    sc.op("dve", lambda: nc.vector.scalar_tensor_tensor(out=st32[h][:], in0=st32[h][:], scalar=float(gam ** 128),
                                                                         in1=ps[4][:, 0:128], op0=ALU.mult, op1=ALU.add),
                          reads=["ps4", "rst32_%d" % h], writes=["rst32_%d" % h])
                    sc.op("act", lambda: nc.scalar.copy(out=stbf[h][:], in_=st32[h][:]), reads=["rst32_%d" % h],
                          writes=["rstbf_%d" % h])
                sc.op("dve", lambda: nc.vector.tensor_copy(out=o32[:], in_=ps[2][:]), reads=["ps2"], writes=["ro32"])
                self.small_stats(o32[:], "ro32", 128.0, EPS)
                sc.dma("pool", g32[:], pT[(33 + h) * 128:(34 + h) * 128, t0:t0 + T], reads=[("pT", 33 + h, ti)], writes=["rg32"])
                sc.op("act", lambda: nc.scalar.activation(out=g32[:], in_=g32[:], func=AF.Silu), reads=["rg32"], writes=["rg32"])
                sc.op("dve", lambda: nc.vector.tensor_tensor(out=o32[:], in0=o32[:], in1=self.mrs[:], op=ALU.mult),
                      reads=["ro32", "mrs"], writes=["ro32"])
                i = (ti * 4 + h) % 2
                sc.op("dve", lambda: nc.vector.tensor_tensor(out=yb[i][:], in0=o32[:], in1=g32[:], op=ALU.mult),
                      reads=["ro32", "rg32"], writes=["ryb%d" % i])
                sc.dma("pool", self.ycatT[1024 + h * 128:1024 + (h + 1) * 128, t0:t0 + T], yb[i][:], reads=["ryb%d" % i],
                       writes=[("ycatT", ti)])


    def rwkv_phase(self, l, st):
        nc, sc, S, I = self.nc, self.sc, self.S, self.I
        NQT = S // T
        NC8 = T // 64
        pT, ps = self.pT, self.ps
        cm = I["cmix"]
        V = nc.vector
        def sbt(name, shape, dt=F32):
            return self.sb(st, "w_" + name, shape, dt)
        msk2 = sbt("msk2", [64, NC8, 128]); mskT = sbt("mskT", [64, NC8, 64]); I8 = sbt("I8", [64, NC8, 64])
        scanm = sbt("scanm", [64, T]); ones64 = sbt("ones64", [64, 64])
        sc.dma("pool", msk2[:], cm[0:64, CM_MSK2:CM_MSK2 + NC8 * 128].rearrange("p (c t) -> p c t", c=NC8), writes=["msk2"])
        sc.dma("pool", mskT[:], cm[0:64, CM_MSKT:CM_MSKT + NC8 * 64].rearrange("p (c t) -> p c t", c=NC8), writes=["mskT"])
        sc.dma("pool", I8[:], cm[0:64, CM_I8:CM_I8 + NC8 * 64].rearrange("p (c t) -> p c t", c=NC8), writes=["I8"])
        sc.dma("pool", scanm[:], cm[0:64, CM_SCAN:CM_SCAN + T], writes=["scanm"])
        sc.op("pool", lambda: nc.gpsimd.memset(ones64[:], 1.0), writes=["ones64"])
        pv = sbt("pv", [64, 6, 8]); pmu = sbt("pmu", [64, 3, 8]); prk = sbt("prk", [64, 8]); pv0 = sbt("pv0", [64, 8])
        omka = sbt("omka", [64, 8])
        muw = sbt("muw", [96, 1]); mua = sbt("mua", [96, 1]); mug = sbt("mug", [128, 2])
        w2s = sbt("w2s", [96, 512]); a2s = sbt("a2s", [96, 512]); g2s = sbt("g2s", [128, 2, 512]); v2s = sbt("v2s", [64, 512])
        mu = I["rwkv_mu"][l]
        with nc.allow_non_contiguous_dma(reason="tiny per-channel parameter vectors"):
            for j in range(6):
                sc.dma("pool", pv[:, j, :], I["rwkv_vec"][l, j].rearrange("(h p) -> p h", p=64), writes=["pv"])
            for j in range(3):
                sc.dma("pool", pmu[:, j, :], mu[j * 512:(j + 1) * 512].rearrange("(h p) -> p h", p=64), writes=["pmu"])
            sc.dma("pool", prk[:], I["rwkv_r_k"][l].rearrange("h p -> p h"), writes=["prk"])
            sc.dma("pool", muw[:], mu[1536:1632].rearrange("(p o) -> p o", o=1), writes=["muw"])
            sc.dma("pool", mua[:], mu[1632:1728].rearrange("(p o) -> p o", o=1), writes=["mua"])
            sc.dma("pool", mug[:], mu[1728:1984].rearrange("(c p) -> p c", p=128), writes=["mug"])
            if l > 0:
                sc.dma("pool", pv0[:], I["rwkv_v0"][l - 1].rearrange("(h p) -> p h", p=64), writes=["pv0"])
        sc.dma("pool", w2s[:], I["rwkv_w2"][l], writes=["w2s"])
        sc.dma("pool", a2s[:], I["rwkv_a2"][l], writes=["a2s"])
        sc.dma("pool", g2s[:], I["rwkv_g2"][l].rearrange("(c p) n -> p c n", p=128), writes=["g2s"])
        if l > 0:
            sc.dma("pool", v2s[:], I["rwkv_v2"][l - 1], writes=["v2s"])
        sc.op("dve", lambda: V.tensor_scalar(out=omka[:], in0=pv[:, 3, :], scalar1=-1.0, scalar2=1.0, op0=ALU.mult, op1=ALU.add),
              ["pv"], ["omka"])
        STs = [sbt("ST%d" % h, [64, 64]) for h in range(8)]
        for h in range(8):
            sc.op("pool", lambda: nc.gpsimd.memset(STs[h][:], 0.0), writes=["ST%d" % h])
        SHARED = {"msk2", "mskT", "I8", "scanm", "ones64", "pv", "pmu", "prk", "pv0", "omka", "muw", "mua", "mug", "w2s", "a2s",
                  "g2s", "v2s", "zwl", "zal", "zgl", "pvr", "cst"} | {"ST%d" % h for h in range(8)}
        rawS = [sbt("rawS%d" % i, [128, T + 1]) for i in range(2)]
        dtmpS = sbt("dtmpS", [128, T])
        zwl = sbt("zwl", [96, T]); zal = sbt("zal", [96, T]); zgl = sbt("zgl", [128, 2, T]); pvr = sbt("pvr", [64, T])
        names = ["r", "k", "v", "ld", "a", "g", "kk", "kkn", "k2", "b", "tt", "Lc", "Lx", "Ep", "Em", "Ex", "kt", "bt", "bon"]
        sets = []
        for si in range(2):
            d = {"id": si}
            d["W"] = {n: sbt("%s_%d" % (n, si), [64, T]) for n in names}
            for n in ("AR", "NA", "KA"):
                d[n] = sbt("%s_%d" % (n, si), [64, NC8, 128])
            for n in ("NT", "Mx", "Nb0", "Nb1", "NTb0", "NTb1", "vtk", "ktk", "btk"):
                d[n] = sbt("%s_%d" % (n, si), [64, NC8, 64])
            d["W0"] = sbt("W0_%d" % si, [64, 64]); d["UT"] = sbt("UT_%d" % si, [64, 64])
            d["raw"] = [sbt("raw%d_%d" % (i, si), [64, T + 1]) for i in range(2)]
            d["dtmp"] = sbt("dtmp_%d" % si, [64, T])
            d["yb"] = sbt("yb_%d" % si, [64, T], BF16)
            d["rr"] = 0
            sets.append(d)
        id64 = self.ident[0:64, 0:64]
        c3 = lambda ap: ap.rearrange("p (c t) -> p c t", t=64)

        def load_shift(raws, rrbox, dtmp_, kx, row0, rows, chunk, ti, mucol, dst, dkey):
            t0 = ti * T
            i = rrbox[0] % 2
            rrbox[0] += 1
            rk = "raw%d" % i
            rd_keys = [("pT", chunk, ti)] + ([("pT", chunk, ti - 1)] if ti > 0 else [])
            if ti == 0:
                sc.op("pool", lambda: nc.gpsimd.memset(raws[i][0:rows, 0:1], 0.0), writes=kx([rk]))
                sc.dma("pool", raws[i][0:rows, 1:T + 1], pT[row0:row0 + rows, 0:T], reads=rd_keys, writes=kx([rk]))
            else:
                sc.dma("pool", raws[i][0:rows, :], pT[row0:row0 + rows, t0 - 1:t0 + T], reads=rd_keys, writes=kx([rk]))
            sc.op("dve", lambda: V.tensor_tensor(out=dtmp_[0:rows, :], in0=raws[i][0:rows, 0:T], in1=raws[i][0:rows, 1:T + 1],
                                                  op=ALU.subtract), kx([rk]), kx(["dtmp"]))
            sc.op("dve", lambda: V.scalar_tensor_tensor(out=dst, in0=dtmp_[0:rows, :], scalar=mucol, in1=raws[i][0:rows, 1:T + 1],
                                                         op0=ALU.mult, op1=ALU.add),
                  kx(["dtmp", rk]) + ["pmu", "muw", "mua", "mug"], kx([dkey]))

        def head_gen(h, ti, D_, pb):
            sfx = "#%d" % D_["id"]
            def kx(keys):
                return [(k + sfx) if (isinstance(k, str) and k not in SHARED and not (k[:2] == "ps" and k[2:].isdigit())) else k
                        for k in keys]
            def dve(fn, r, w):
                return sc.op("dve", fn, kx(r), kx(w))
            def act(fn, r, w):
                return sc.op("act", fn, kx(r), kx(w))
            def pe(fn, r, w):
                return sc.op("pe", fn, kx(r), kx(w))
            P = lambda i: ps[pb + i]
            PK = lambda i: "ps%d" % (pb + i)
            t0 = ti * T
            W = D_["W"]
            AR, NA, KA, NT, Mx = D_["AR"], D_["NA"], D_["KA"], D_["NT"], D_["Mx"]
            Nb, NTb = [D_["Nb0"], D_["Nb1"]], [D_["NTb0"], D_["NTb1"]]
            vtk, ktk, btk, W0, UT = D_["vtk"], D_["ktk"], D_["btk"], D_["W0"], D_["UT"]
            rrbox = [D_["rr"]]
            hc = slice(h * 64, (h + 1) * 64)
            ro = (h % 2) * 64
            vec = lambda j: pv[:, j, h:h + 1]
            r, k, v, ld, a, g = W["r"], W["k"], W["v"], W["ld"], W["a"], W["g"]
            ls = lambda *args: load_shift(D_["raw"], rrbox, D_["dtmp"], kx, *args)
            ls((37 + h // 2) * 128 + ro, 64, 37 + h // 2, ti, pmu[:, 0, h:h + 1], r[:], "r")
            yield
            ls((41 + h // 2) * 128 + ro, 64, 41 + h // 2, ti, pmu[:, 1, h:h + 1], k[:], "k")
            yield
            ls((45 + h // 2) * 128 + ro, 64, 45 + h // 2, ti, pmu[:, 2, h:h + 1], v[:], "v")
            D_["rr"] = rrbox[0]
            yield
            p3, k3 = P(3), PK(3)
            if l == 0:
                sc.dma("pool", self.vfirstT[hc, t0:t0 + T], v[:], reads=kx(["v"]), writes=[("vfirst", h, ti)])
            else:
                vf = W["Lc"]
                pe(lambda: nc.tensor.matmul(p3[0:64, :], lhsT=v2s[:, hc], rhs=pvr[:], start=True, stop=True), ["v2s", "pvr"], [k3])
                act(lambda: nc.scalar.activation(out=W["tt"][:], in_=p3[0:64, :], func=AF.Sigmoid, bias=pv0[:, h:h + 1]),
                    [k3, "pv0"], ["tt"])
                sc.dma("pool", vf[:], self.vfirstT[hc, t0:t0 + T], reads=[("vfirst", h, ti)], writes=kx(["Lc"]))
                yield
                dve(lambda: V.tensor_tensor(out=vf[:], in0=vf[:], in1=v[:], op=ALU.subtract), ["Lc", "v"], ["Lc"])
                yield
                dve(lambda: V.tensor_tensor(out=vf[:], in0=vf[:], in1=W["tt"][:], op=ALU.mult), ["Lc", "tt"], ["Lc"])
                yield
                dve(lambda: V.tensor_tensor(out=v[:], in0=v[:], in1=vf[:], op=ALU.add), ["Lc", "v"], ["v"])
            yield
            pe(lambda: nc.tensor.matmul(p3[0:64, :], lhsT=w2s[:, hc], rhs=zwl[:], start=True, stop=True), ["w2s", "zwl"], [k3])
            act(lambda: nc.scalar.activation(out=ld[:], in_=p3[0:64, :], func=AF.Sigmoid, bias=vec(0)), [k3, "pv"], ["ld"])
            yield
            dve(lambda: V.tensor_scalar(out=ld[:], in0=ld[:], scalar1=-0.6065306597126334, scalar2=None, op0=ALU.mult), ["ld"], ["ld"])
            pe(lambda: nc.tensor.matmul(p3[0:64, :], lhsT=a2s[:, hc], rhs=zal[:], start=True, stop=True), ["a2s", "zal"], [k3])
            act(lambda: nc.scalar.activation(out=a[:], in_=p3[0:64, :], func=AF.Sigmoid, bias=vec(1)), [k3, "pv"], ["a"])
            yield
            for gc in range(2):
                pe(lambda: nc.tensor.matmul(p3[0:64, :], lhsT=g2s[:, gc, hc], rhs=zgl[:, gc, :], start=(gc == 0), stop=(gc == 1)),
                   ["g2s", "zgl"], [k3])
            act(lambda: nc.scalar.copy(out=g[:], in_=p3[0:64, :]), [k3], ["g"])
            yield
            kk, kkn, k2, b, tt = W["kk"], W["kkn"], W["k2"], W["b"], W["tt"]
            dve(lambda: V.tensor_scalar(out=kk[:], in0=k[:], scalar1=vec(2), scalar2=None, op0=ALU.mult), ["k", "pv"], ["kk"])
            yield
            dve(lambda: V.tensor_tensor(out=tt[:], in0=kk[:], in1=kk[:], op=ALU.mult), ["kk"], ["tt"])
            yield
            pe(lambda: nc.tensor.matmul(p3[0:64, :], lhsT=ones64[:], rhs=tt[:], start=True, stop=True), ["ones64", "tt"], [k3])
            act(lambda: nc.scalar.activation(out=tt[:], in_=p3[0:64, :], func=AF.Sqrt), [k3], ["tt"])
            yield
            dve(lambda: V.tensor_scalar(out=tt[:], in0=tt[:], scalar1=1e-12, scalar2=None, op0=ALU.max), ["tt"], ["tt"])
            yield
            dve(lambda: V.reciprocal(out=tt[:], in_=tt[:]), ["tt"], ["tt"])
            yield
            dve(lambda: V.tensor_tensor(out=kkn[:], in0=kk[:], in1=tt[:], op=ALU.mult), ["kk", "tt"], ["kkn"])
            yield
            dve(lambda: V.tensor_scalar(out=tt[:], in0=a[:], scalar1=vec(3), scalar2=omka[:, h:h + 1], op0=ALU.mult, op1=ALU.add),
                ["a", "pv", "omka"], ["tt"])
            yield
            dve(lambda: V.tensor_tensor(out=k2[:], in0=k[:], in1=tt[:], op=ALU.mult), ["k", "tt"], ["k2"])
            yield
            dve(lambda: V.tensor_tensor(out=b[:], in0=kkn[:], in1=a[:], op=ALU.mult), ["kkn", "a"], ["b"])
            yield
            dve(lambda: V.scalar_tensor_tensor(out=tt[:], in0=r[:], scalar=prk[:, h:h + 1], in1=k2[:], op0=ALU.mult, op1=ALU.mult),
                ["r", "k2", "prk"], ["tt"])
            yield
            pe(lambda: nc.tensor.matmul(p3[0:64, :], lhsT=ones64[:], rhs=tt[:], start=True, stop=True), ["ones64", "tt"], [k3])
            dve(lambda: V.tensor_tensor(out=W["bon"][:], in0=p3[0:64, :], in1=v[:], op=ALU.mult), [k3, "v"], ["bon"])
            yield
            Lc, Lx, Ep, Em, Ex, kt, bt = W["Lc"], W["Lx"], W["Ep"], W["Em"], W["Ex"], W["kt"], W["bt"]
            dve(lambda: V.tensor_tensor_scan(out=Lc[:], data0=scanm[:], data1=ld[:], initial=0.0, op0=ALU.mult, op1=ALU.add),
                ["scanm", "ld"], ["Lc"])
            yield
            dve(lambda: V.tensor_tensor(out=Lx[:], in0=Lc[:], in1=ld[:], op=ALU.subtract), ["Lc", "ld"], ["Lx"])
            act(lambda: nc.scalar.activation(out=Ep[:], in_=Lc[:], func=AF.Exp), ["Lc"], ["Ep"])
            yield
            act(lambda: nc.scalar.activation(out=Em[:], in_=Lc[:], func=AF.Exp, scale=-1.0), ["Lc"], ["Em"])
            yield
            act(lambda: nc.scalar.activation(out=Ex[:], in_=Lx[:], func=AF.Exp), ["Lx"], ["Ex"])
            yield
            dve(lambda: V.scalar_tensor_tensor(out=AR[:, :, 0:64], in0=c3(kkn[:]), scalar=-1.0, in1=c3(Ex[:]), op0=ALU.mult, op1=ALU.mult),
                ["kkn", "Ex"], ["AR"])
            yield
            dve(lambda: V.tensor_tensor(out=AR[:, :, 64:128], in0=c3(r[:]), in1=c3(Ep[:]), op=ALU.mult), ["r", "Ep"], ["AR"])
            yield
            dve(lambda: V.tensor_tensor(out=kt[:], in0=k2[:], in1=Em[:], op=ALU.mult), ["k2", "Em"], ["kt"])
            yield
            dve(lambda: V.tensor_tensor(out=bt[:], in0=b[:], in1=Em[:], op=ALU.mult), ["b", "Em"], ["bt"])
            yield
            for c in range(NC8):
                cc = slice(c * 64, (c + 1) * 64)
                bk, co = c // 4, (c % 4) * 128
                pe(lambda: nc.tensor.matmul(P(bk)[0:64, co:co + 128], lhsT=bt[:, cc], rhs=AR[:, c, :], start=True, stop=True),
                   ["bt", "AR"], [PK(bk)])
                pe(lambda: nc.tensor.matmul(P(2)[0:64, cc], lhsT=AR[:, c, 0:64], rhs=bt[:, cc], start=True, stop=True),
                   ["bt", "AR"], [PK(2)])
                if c % 2 == 1:
                    yield
            for bk in range(2):
                cs_ = slice(bk * 4, bk * 4 + 4)
                dve(lambda: V.tensor_tensor(out=NA[:, cs_, :], in0=P(bk)[0:64, :].rearrange("p (c t) -> p c t", t=128),
                                            in1=msk2[:, cs_, :], op=ALU.mult), [PK(bk), "msk2"], ["NA"])
                yield
            dve(lambda: V.tensor_tensor(out=NT[:], in0=c3(P(2)[0:64, :]), in1=mskT[:], op=ALU.mult), [PK(2), "mskT"], ["NT"])
            yield
            for c in range(NC8):
                cc = slice(c * 64, (c + 1) * 64)
                bk, co = c // 4, (c % 4) * 128
                pe(lambda: nc.tensor.matmul(P(bk)[0:64, co:co + 128], lhsT=kt[:, cc], rhs=AR[:, c, :], start=True, stop=True),
                   ["kt", "AR"], [PK(bk)])
                if c % 2 == 1:
                    yield
            for bk in range(2):
                cs_ = slice(bk * 4, bk * 4 + 4)
                dve(lambda: V.tensor_tensor(out=KA[:, cs_, :], in0=P(bk)[0:64, :].rearrange("p (c t) -> p c t", t=128),
                                            in1=msk2[:, cs_, :], op=ALU.mult), [PK(bk), "msk2"], ["KA"])
                yield
            dve(lambda: V.tensor_tensor(out=Mx[:], in0=NA[:, :, 0:64], in1=I8[:], op=ALU.add), ["NA", "I8"], ["Mx"])
            yield
            curN, curNT, kN, kNT = NA[:, :, 0:64], NT[:], "NA", "NT"
            for rnd in range(5):
                i = rnd % 2
                lastr = (rnd == 4)
                for c in range(NC8):
                    cc = slice(c * 64, (c + 1) * 64)
                    if not lastr:
                        pe(lambda: nc.tensor.matmul(P(0)[0:64, cc], lhsT=curNT[:, c, :], rhs=curN[:, c, :], start=True, stop=True),
                           [kN, kNT], [PK(0)])
                    pe(lambda: nc.tensor.matmul(P(1)[0:64, cc], lhsT=curN[:, c, :], rhs=curNT[:, c, :], start=True, stop=True),
                       [kN, kNT], [PK(1)])
                    if c % 2 == 1:
                        yield
                if not lastr:
                    act(lambda: nc.scalar.copy(out=Nb[i][:], in_=c3(P(0)[0:64, :])), [PK(0)], ["Nb%d" % i])
                dve(lambda: V.tensor_copy(out=NTb[i][:], in_=c3(P(1)[0:64, :])), [PK(1)], ["NTb%d" % i])
                yield
                for c in range(NC8):
                    cc = slice(c * 64, (c + 1) * 64)
                    pe(lambda: nc.tensor.matmul(P(2)[0:64, cc], lhsT=NTb[i][:, c, :], rhs=Mx[:, c, :], start=True, stop=True),
                       ["NTb%d" % i, "Mx"], [PK(2)])
                    if c % 4 == 3:
                        yield
                dve(lambda: V.tensor_tensor(out=Mx[:], in0=Mx[:], in1=c3(P(2)[0:64, :]), op=ALU.add), [PK(2), "Mx"], ["Mx"])
                yield
                curN, curNT, kN, kNT = Nb[i][:], NTb[i][:], "Nb%d" % i, "NTb%d" % i
            for (src, sk, dst, dk, bk) in ((v, "v", vtk, "vtk", 0), (kt, "kt", ktk, "ktk", 1), (bt, "bt", btk, "btk", 2)):
                for c in range(NC8):
                    cc = slice(c * 64, (c + 1) * 64)
                    pe(lambda: nc.tensor.transpose(out=P(bk)[0:64, cc], in_=src[:, cc], identity=id64), [sk, "cst"], [PK(bk)])
                yield
                act(lambda: nc.scalar.copy(out=dst[:], in_=c3(P(bk)[0:64, :])), [PK(bk)], [dk])
                yield
            ST, sk_ = STs[h], "ST%d" % h
            for c in range(NC8):
                cc = slice(c * 64, (c + 1) * 64)
                pe(lambda: nc.tensor.matmul(P(0)[0:64, 0:64], lhsT=AR[:, c, 0:64], rhs=ST[:], start=True, stop=False), ["AR", sk_], [PK(0)])
                pe(lambda: nc.tensor.matmul(P(0)[0:64, 0:64], lhsT=KA[:, c, 0:64], rhs=vtk[:, c, :], start=False, stop=True),
                   ["KA", "vtk"], [PK(0)])
                yield
                act(lambda: nc.scalar.copy(out=W0[:], in_=P(0)[0:64, 0:64]), [PK(0)], ["W0"])
                yield
                pe(lambda: nc.tensor.matmul(P(1)[0:64, 0:64], lhsT=Mx[:, c, :], rhs=W0[:], start=True, stop=True), ["Mx", "W0"], [PK(1)])
                yield
                act(lambda: nc.scalar.copy(out=UT[:], in_=P(1)[0:64, 0:64]), [PK(1)], ["UT"])
                yield
                pe(lambda: nc.tensor.matmul(P(3)[0:64, 0:64], lhsT=btk[:, c, :], rhs=UT[:], start=True, stop=False), ["btk", "UT"], [PK(3)])
                pe(lambda: nc.tensor.matmul(P(3)[0:64, 0:64], lhsT=ktk[:, c, :], rhs=vtk[:, c, :], start=False, stop=True),
                   ["ktk", "vtk"], [PK(3)])
                pe(lambda: nc.tensor.matmul(P(2)[0:64, cc], lhsT=ST[:], rhs=AR[:, c, 64:128], start=True, stop=False), [sk_, "AR"], [PK(2)])
                pe(lambda: nc.tensor.matmul(P(2)[0:64, cc], lhsT=UT[:], rhs=NA[:, c, 64:128], start=False, stop=False), ["UT", "NA"], [PK(2)])
                pe(lambda: nc.tensor.matmul(P(2)[0:64, cc], lhsT=vtk[:, c, :], rhs=KA[:, c, 64:128], start=False, stop=True),
                   ["vtk", "KA"], [PK(2)])
                yield
                dve(lambda: V.tensor_tensor(out=ST[:], in0=ST[:], in1=P(3)[0:64, 0:64], op=ALU.add), [sk_, PK(3)], [sk_])
                yield
                dve(lambda: V.tensor_scalar(out=ST[:], in0=ST[:], scalar1=Ep[:, c * 64 + 63:c * 64 + 64], scalar2=None, op0=ALU.mult),
                    [sk_, "Ep"], [sk_])
                yield
            y, ysq, mean, msq2, rs = W["kk"], W["kkn"], W["Lx"], W["Ex"], W["Em"]
            act(lambda: nc.scalar.copy(out=y[:], in_=P(2)[0:64, :]), [PK(2)], ["kk"])
            yield
            pe(lambda: nc.tensor.matmul(P(0)[0:64, :], lhsT=ones64[:], rhs=y[:], start=True, stop=True), ["ones64", "kk"], [PK(0)])
            dve(lambda: V.tensor_tensor(out=ysq[:], in0=y[:], in1=y[:], op=ALU.mult), ["kk"], ["kkn"])
            yield
            pe(lambda: nc.tensor.matmul(P(1)[0:64, :], lhsT=ones64[:], rhs=ysq[:], start=True, stop=True), ["ones64", "kkn"], [PK(1)])
            dve(lambda: V.tensor_scalar(out=mean[:], in0=P(0)[0:64, :], scalar1=1.0 / 64, scalar2=None, op0=ALU.mult), [PK(0)], ["Lx"])
            yield
            dve(lambda: V.tensor_tensor(out=msq2[:], in0=mean[:], in1=mean[:], op=ALU.mult), ["Lx"], ["Ex"])
            yield
            dve(lambda: V.scalar_tensor_tensor(out=rs[:], in0=P(1)[0:64, :], scalar=1.0 / 64, in1=msq2[:], op0=ALU.mult, op1=ALU.subtract),
                [PK(1), "Ex"], ["Em"])
            yield
            dve(lambda: V.tensor_scalar(out=rs[:], in0=rs[:], scalar1=64e-5, scalar2=None, op0=ALU.add), ["Em"], ["Em"])
            yield
            act(lambda: nc.scalar.activation(out=rs[:], in_=rs[:], func=AF.Sqrt), ["Em"], ["Em"])
            yield
            dve(lambda: V.reciprocal(out=rs[:], in_=rs[:]), ["Em"], ["Em"])
            yield
            dve(lambda: V.tensor_tensor(out=y[:], in0=y[:], in1=mean[:], op=ALU.subtract), ["kk", "Lx"], ["kk"])
            yield
            dve(lambda: V.tensor_tensor(out=y[:], in0=y[:], in1=rs[:], op=ALU.mult), ["kk", "Em"], ["kk"])
            yield
            dve(lambda: V.tensor_scalar(out=y[:], in0=y[:], scalar1=vec(4), scalar2=vec(5), op0=ALU.mult, op1=ALU.add), ["kk", "pv"], ["kk"])
            yield
            dve(lambda: V.tensor_tensor(out=y[:], in0=y[:], in1=W["bon"][:], op=ALU.add), ["kk", "bon"], ["kk"])
            yield
            dve(lambda: V.tensor_tensor(out=D_["yb"][:], in0=y[:], in1=g[:], op=ALU.mult), ["kk", "g"], ["yb"])
            sc.dma("pool", self.ycatT[1536 + h * 64:1536 + (h + 1) * 64, t0:t0 + T], D_["yb"][:], reads=kx(["yb"]),
                   writes=[("ycatT", ti)])
            yield

        srr = [0]
        ident_kx = lambda keys: list(keys)
        for ti in range(NQT):
            t0 = ti * T
            load_shift(rawS, srr, dtmpS, ident_kx, 49 * 128, 96, 49, ti, muw[:, 0:1], zwl[:], "zwl")
            sc.op("act", lambda: nc.scalar.activation(out=zwl[:], in_=zwl[:], func=AF.Tanh), ["zwl"], ["zwl"])
            load_shift(rawS, srr, dtmpS, ident_kx, 50 * 128, 96, 50, ti, mua[:, 0:1], zal[:], "zal")
            for gc in range(2):
                load_shift(rawS, srr, dtmpS, ident_kx, (51 + gc) * 128, 128, 51 + gc, ti, mug[:, gc:gc + 1], zgl[:, gc, :], "zgl")
            sc.op("act", lambda: nc.scalar.activation(out=zgl[:], in_=zgl[:], func=AF.Sigmoid), ["zgl"], ["zgl"])
            if l > 0:
                sc.dma("pool", pvr[:], pT[53 * 128:53 * 128 + 64, t0:t0 + T], reads=[("pT", 53, ti)], writes=["pvr"])
            for hp in range(4):
                gens = [head_gen(2 * hp, ti, sets[0], 0), head_gen(2 * hp + 1, ti, sets[1], 4)]
                while gens:
                    for gobj in list(gens):
                        try:
                            next(gobj)
                        except StopIteration:
                            gens.remove(gobj)


C_ID = 0
C_ONE = 128
CST_W = 129
CM_MASK = 0
CM_RM = CM_MASK + 4 * T
CM_DIN = CM_RM + 128
CM_QDEC = CM_DIN + 512
CM_KDEC = CM_QDEC + 4 * T
CM_MSK2 = CM_KDEC + 4
CM_MSKT = CM_MSK2 + 8 * 128
CM_I8 = CM_MSKT + 8 * 64
CM_SCAN = CM_I8 + 8 * 64
CM_W = CM_SCAN + T
RET_GAMMA = [float(1.0 - 2.0 ** (-5.0 - h)) for h in range(4)]


def make_consts(S):
    c = np.zeros((128, CST_W), np.float32)
    c[:, C_ID:C_ID + 128] = np.eye(128, dtype=np.float32)
    c[:, C_ONE] = 1.0
    cm = np.zeros((128, CM_W), np.float32)
    p = np.arange(128)[:, None]
    col = np.arange(T)[None, :]
    for j in range(4):
        cm[:, CM_MASK + j * T:CM_MASK + (j + 1) * T] = np.where(col >= j * 128 + p, 0.0, -30000.0)
    rm = np.zeros((128, 128), np.float32)
    for m in range(64):
        rm[m + 64, m] = -1.0
        rm[m, m + 64] = 1.0
    cm[:, CM_RM:CM_RM + 128] = rm
    scale = 128.0 ** -0.5
    i = np.arange(128)
    for h in range(4):
        lgm = np.log(np.float64(RET_GAMMA[h]))
        diff = i[None, :] - i[:, None]
        cm[:, CM_DIN + h * 128:CM_DIN + (h + 1) * 128] = np.where(diff >= 0, scale * np.exp(np.maximum(diff, 0) * lgm), 0.0)
        cm[:, CM_QDEC + h * T:CM_QDEC + (h + 1) * T] = np.tile(scale * np.exp((i + 1.0) * lgm), T // 128)[None, :]
        cm[:, CM_KDEC + h] = np.exp((127.0 - i) * lgm)
    ii = np.arange(64)[:, None]
    tt = np.arange(64)[None, :]
    su = (ii < tt).astype(np.float32)
    ui = (ii <= tt).astype(np.float32)
    sl = (ii > tt).astype(np.float32)
    for c8 in range(8):
        cm[0:64, CM_MSK2 + c8 * 128:CM_MSK2 + c8 * 128 + 64] = su
        cm[0:64, CM_MSK2 + c8 * 128 + 64:CM_MSK2 + (c8 + 1) * 128] = ui
        cm[0:64, CM_MSKT + c8 * 64:CM_MSKT + (c8 + 1) * 64] = sl
        cm[0:64, CM_I8 + c8 * 64:CM_I8 + (c8 + 1) * 64] = np.eye(64, dtype=np.float32)
    cm[:, CM_SCAN:CM_SCAN + T] = (np.arange(T) % 64 != 0).astype(np.float32)[None, :]
    half = 64
    inv = (10000.0 ** (-np.arange(half, dtype=np.float32) / half)).astype(np.float32)
    ang = (np.arange(S, dtype=np.float32)[None, :] * inv[:, None]).astype(np.float32)
    rot = np.empty((2, 128, S), np.float32)
    rot[0, :64] = np.cos(ang); rot[0, 64:] = np.cos(ang)
    rot[1, :64] = np.sin(ang); rot[1, 64:] = np.sin(ang)
    return c, cm, rot


_CACHE = {}


def get_program(S, nq, **kw):
    key = (S, nq, tuple(sorted(kw.items())))
    if key not in _CACHE:
        _CACHE[key] = Builder(S, nq, **kw).build()
    return _CACHE[key]


def kernel(**inputs):
    x = np.asarray(inputs["x"], np.float32)
    B, S, _ = x.shape
    nq = 4
    nc = get_program(S, nq)
    cst, cmix, rot = make_consts(S)
    in_maps = []
    for c in range(8):
        b = c // nq
        m = {k: np.ascontiguousarray(np.asarray(v, np.float32)) for k, v in inputs.items() if k != "x"}
        m["x"] = np.ascontiguousarray(x[b])
        m["cst"] = cst
        m["cmix"] = cmix
        m["rot"] = rot
        in_maps.append(m)
    res = run_bass_kernel_spmd(nc, in_maps, core_ids=list(range(8)))
    out = np.empty((B, S, x.shape[2]), np.float32)
    q = S // nq
    for c in range(8):
        b, k = c // nq, c % nq
        out[b, k * q:(k + 1) * q] = res.results[c]["out"]
    return out
```
